# Optimizing a Trainium2 kernel written in Bass

```python
import jax
import jax.numpy as jnp
from jax import lax
import numpy as np

D_MODEL = 1024
BATCH = 8
SEQ = 4096
DEPTH = 4

HEAD_DIM = 64
N_MIXERS = 4
GROUP_W = D_MODEL // N_MIXERS
GROUP_HEADS = GROUP_W // HEAD_DIM
CONV_K = 31
GLA_DK = HEAD_DIM // 2
GLA_RANK = 16
GLA_TAU = 16.0
GLA_CHUNK = 64
DIL_PAIRS = ((128, 1), (512, 4), (2048, 16))
DIL_BLOCK = 128
ROPE_THETA = 10000.0
SGU_CHUNK = 128
D_FF = 2816
FFN_CONV_K = 3
EPS = 1e-6
NEG_INF = -1e30

IN_WIDTHS = (
    GROUP_W, GROUP_W,
    GROUP_HEADS * GLA_DK, GROUP_HEADS * GLA_DK,
    GROUP_W, GROUP_W, GLA_RANK,
    GROUP_W, GROUP_W, GROUP_W,
    GROUP_W, GROUP_W,
)
N_IN = sum(IN_WIDTHS)

kernel_name = 'hybrid_four_mixer_block'


def rmsnorm(x, g):
    x32 = x.astype(jnp.float32)
    y = x32 * lax.rsqrt(jnp.mean(x32 * x32, axis=-1, keepdims=True) + EPS)
    return (y * g.astype(jnp.float32)).astype(x.dtype)


def layernorm(x, g, b):
    x32 = x.astype(jnp.float32)
    mu = jnp.mean(x32, axis=-1, keepdims=True)
    var = jnp.mean(jnp.square(x32 - mu), axis=-1, keepdims=True)
    y = (x32 - mu) * lax.rsqrt(var + EPS)
    return (y * g.astype(jnp.float32) + b.astype(jnp.float32)).astype(x.dtype)


def causal_dwconv(x, w, b):
    K, C = w.shape
    xp = jnp.pad(x, ((0, 0), (K - 1, 0), (0, 0)))
    y = lax.conv_general_dilated(xp, w[:, None, :].astype(x.dtype), window_strides=(1,),
                                 padding='VALID', dimension_numbers=('NWC', 'WIO', 'NWC'),
                                 feature_group_count=C)
    return y + b.astype(x.dtype)


def rotary(t, cos, sin):
    t1, t2 = jnp.split(t, 2, axis=-1)
    return jnp.concatenate([t1 * cos - t2 * sin, t2 * cos + t1 * sin], axis=-1)


def conformer_conv(a_val, a_gate, conv_w, conv_b, ln_g, ln_b):
    h = a_val * jax.nn.sigmoid(a_gate)
    h = causal_dwconv(h, conv_w, conv_b)
    h = layernorm(h, ln_g, ln_b)
    return jax.nn.silu(h)


def gla_chunked(q, k, v, gk):
    B, S, H, dk = q.shape
    dv = v.shape[-1]
    nc = S // GLA_CHUNK

    def chunks(t):
        return t.reshape(B, nc, GLA_CHUNK, H, t.shape[-1]).transpose(1, 0, 3, 2, 4)

    q, k, v, gk = chunks(q), chunks(k), chunks(v), chunks(gk)
    b = jnp.cumsum(gk, axis=3)
    b_last = b[:, :, :, -1:, :]
    q_t = q * jnp.exp(b)
    k_t = k * jnp.exp(-b)
    k_end = k * jnp.exp(b_last - b)
    causal = jnp.tril(jnp.ones((GLA_CHUNK, GLA_CHUNK), dtype=bool))
    att = jnp.where(causal, jnp.einsum('nbhie,nbhje->nbhij', q_t, k_t), 0.0)
    o_intra = jnp.einsum('nbhij,nbhjv->nbhiv', att, v)

    def step(state, inp):
        k_e, v_c, dec = inp
        new = dec[:, :, 0, :, None] * state + jnp.einsum('bhje,bhjv->bhev', k_e, v_c)
        return new, state

    s0 = jnp.zeros((B, H, dk, dv), jnp.float32)
    _, s_prev = lax.scan(step, s0, (k_end, v, jnp.exp(b_last)))
    o_inter = jnp.einsum('nbhie,nbhev->nbhiv', q_t, s_prev)
    return (o_intra + o_inter).transpose(1, 0, 3, 2, 4).reshape(B, S, H, dv)


def gla_mixer(q, k, v, g, lr, w2, gb, norm_g):
    B, S, _ = q.shape
    f32 = jnp.float32
    q = q.reshape(B, S, GROUP_HEADS, GLA_DK).astype(f32) * GLA_DK ** -0.5
    k = k.reshape(B, S, GROUP_HEADS, GLA_DK).astype(f32)
    v = v.reshape(B, S, GROUP_HEADS, HEAD_DIM).astype(f32)
    gk = jax.nn.log_sigmoid((lr @ w2 + gb).astype(f32)) / GLA_TAU
    o = gla_chunked(q, k, v, gk.reshape(B, S, GROUP_HEADS, GLA_DK))
    o = o * lax.rsqrt(jnp.mean(o * o, axis=-1, keepdims=True) + EPS) * norm_g.astype(f32)
    o = o.reshape(B, S, GROUP_W) * jax.nn.silu(g.astype(f32))
    return o.astype(g.dtype)


def dilated_branch(q, k, v, dil, back):
    B, S, H, E = q.shape
    L = S // dil
    nb = -(-L // DIL_BLOCK)
    Lp = nb * DIL_BLOCK

    def strided(t):
        t = t.reshape(B, L, dil, H, E).transpose(0, 2, 3, 1, 4)
        t = jnp.pad(t, ((0, 0), (0, 0), (0, 0), (0, Lp - L), (0, 0)))
        return t.reshape(B, dil, H, nb, DIL_BLOCK, E)

    def with_prev(t):
        prev = jnp.pad(t[:, :, :, :-1], ((0, 0), (0, 0), (0, 0), (1, 0), (0, 0), (0, 0)))
        return jnp.concatenate([prev, t], axis=4)

    qs = strided(q)
    kk = with_prev(strided(k))
    vv = with_prev(strided(v))
    s = jnp.einsum('bdhnqe,bdhnke->bdhnqk', qs, kk)
    i = jnp.arange(DIL_BLOCK)[:, None]
    j = jnp.arange(2 * DIL_BLOCK)[None, :]
    rel = i - j + DIL_BLOCK
    band = (rel >= 0) & (rel <= back)
    valid = (jnp.arange(nb)[:, None, None] > 0) | (j[None] >= DIL_BLOCK)
    mask = band[None] & valid
    s = jnp.where(mask, s, NEG_INF)
    m = jnp.max(s, axis=-1, keepdims=True)
    p = jnp.exp(s - m)
    den = jnp.sum(p, axis=-1, keepdims=True)
    o = jnp.einsum('bdhnqk,bdhnke->bdhnqe', p, vv) / den
    lse = m + jnp.log(den)

    def unstride(t):
        t = t.reshape(B, dil, H, Lp, t.shape[-1])[:, :, :, :L]
        return t.transpose(0, 3, 1, 2, 4).reshape(B, S, H, t.shape[-1])

    return unstride(o), unstride(lse)[..., 0]


def dilated_attention(q, k, v, cos, sin):
    B, S, _ = q.shape
    shp = (B, S, GROUP_HEADS, HEAD_DIM)
    f32 = jnp.float32
    q = rotary(q.reshape(shp).astype(f32), cos, sin) * HEAD_DIM ** -0.5
    k = rotary(k.reshape(shp).astype(f32), cos, sin)
    v = v.reshape(shp).astype(f32)
    outs, lses = [], []
    for window, dil in DIL_PAIRS:
        o, lse = dilated_branch(q, k, v, dil, window // dil)
        outs.append(o)
        lses.append(lse)
    w = jax.nn.softmax(jnp.stack(lses), axis=0)
    o = jnp.einsum('nbsh,nbshe->bshe', w, jnp.stack(outs))
    return o.reshape(B, S, GROUP_W)


def sgu_mixer(u, v, ln_g, ln_b, w_s, b_s):
    u = jax.nn.gelu(u)
    v = layernorm(jax.nn.gelu(v), ln_g, ln_b)
    B, S, _ = v.shape
    n = S // SGU_CHUNK
    vc = v.reshape(B, n, SGU_CHUNK, GROUP_HEADS, HEAD_DIM)
    causal = jnp.tril(jnp.ones((SGU_CHUNK, SGU_CHUNK), dtype=bool))
    w = jnp.where(causal[None], w_s, 0.0).astype(v.dtype)
    sv = jnp.einsum('gts,bnsge->bntge', w, vc) + b_s.T[:, :, None].astype(v.dtype)
    return u * sv.reshape(B, S, GROUP_W)


def conv_ffn(h, w_up, conv_w, conv_b, w_down):
    z = causal_dwconv(h @ w_up, conv_w, conv_b)
    a, b = jnp.split(z, 2, axis=-1)
    return (jax.nn.silu(a) * b) @ w_down


def setup_inputs(seed: int = 0) -> dict:
    key = jax.random.key(seed)
    ks = jax.random.split(key, 24)
    L = DEPTH

    def nrm(k, shape, scale):
        return jax.random.normal(k, shape, jnp.float32) * scale

    def gain(k, shape):
        return 1.0 + 0.02 * jax.random.normal(k, shape, jnp.float32)

    return {
        'x': nrm(ks[0], (BATCH, SEQ, D_MODEL), 1.0),
        'c': nrm(ks[1], (BATCH, D_MODEL), 1.0),
        'ada_w': nrm(ks[2], (L, D_MODEL, 6 * D_MODEL), 0.5 * D_MODEL ** -0.5),
        'ada_b': nrm(ks[3], (L, 6 * D_MODEL), 0.02),
        'ln1_g': gain(ks[4], (L, D_MODEL)),
        'w_in': nrm(ks[5], (L, D_MODEL, N_IN), D_MODEL ** -0.5),
        'conv_w': nrm(ks[6], (L, CONV_K, GROUP_W), CONV_K ** -0.5),
        'conv_b': nrm(ks[7], (L, GROUP_W), 0.02),
        'cln_g': gain(ks[8], (L, GROUP_W)),
        'cln_b': nrm(ks[9], (L, GROUP_W), 0.02),
        'gla_w2': nrm(ks[10], (L, GLA_RANK, GROUP_HEADS * GLA_DK), GLA_RANK ** -0.5),
        'gla_b': nrm(ks[11], (L, GROUP_HEADS * GLA_DK), 0.02),
        'gla_norm_g': gain(ks[12], (L, HEAD_DIM)),
        'sgu_ln_g': gain(ks[13], (L, GROUP_W)),
        'sgu_ln_b': nrm(ks[14], (L, GROUP_W), 0.02),
        'sgu_w': nrm(ks[15], (L, GROUP_HEADS, SGU_CHUNK, SGU_CHUNK), 0.5 * SGU_CHUNK ** -0.5),
        'sgu_b': gain(ks[16], (L, GROUP_HEADS, SGU_CHUNK)),
        'w_out': nrm(ks[17], (L, D_MODEL, D_MODEL), D_MODEL ** -0.5),
        'ln2_g': gain(ks[18], (L, D_MODEL)),
        'ffn_up': nrm(ks[19], (L, D_MODEL, 2 * D_FF), D_MODEL ** -0.5),
        'ffn_conv_w': nrm(ks[20], (L, FFN_CONV_K, 2 * D_FF), FFN_CONV_K ** -0.5),
        'ffn_conv_b': nrm(ks[21], (L, 2 * D_FF), 0.02),
        'ffn_down': nrm(ks[22], (L, D_FF, D_MODEL), D_FF ** -0.5),
        'lnf_g': gain(ks[23], (D_MODEL,)),
    }


def reference(x, c, ada_w, ada_b, ln1_g, w_in, conv_w, conv_b, cln_g, cln_b,
              gla_w2, gla_b, gla_norm_g, sgu_ln_g, sgu_ln_b, sgu_w, sgu_b, w_out,
              ln2_g, ffn_up, ffn_conv_w, ffn_conv_b, ffn_down, lnf_g):
    B, S, _ = x.shape
    dt = x.dtype
    inv = 1.0 / (ROPE_THETA ** (jnp.arange(0, HEAD_DIM, 2, dtype=jnp.float32) / HEAD_DIM))
    ang = jnp.arange(S, dtype=jnp.float32)[:, None] * inv[None, :]
    cos = jnp.cos(ang)[None, :, None, :]
    sin = jnp.sin(ang)[None, :, None, :]
    cond = jax.nn.silu(c)
    splits = np.cumsum(IN_WIDTHS)[:-1].tolist()

    for l in range(DEPTH):
        mod = cond @ ada_w[l] + ada_b[l]
        sh1, sc1, g1, sh2, sc2, g2 = [m[:, None, :] for m in jnp.split(mod, 6, axis=-1)]

        h = rmsnorm(x, ln1_g[l]) * (1 + sc1) + sh1
        (a_val, a_gate, b_q, b_k, b_v, b_g, b_lr,
         c_q, c_k, c_v, d_u, d_v) = jnp.split(h @ w_in[l], splits, axis=-1)
        o_a = conformer_conv(a_val, a_gate, conv_w[l], conv_b[l], cln_g[l], cln_b[l])
        o_b = gla_mixer(b_q, b_k, b_v, b_g, b_lr, gla_w2[l], gla_b[l], gla_norm_g[l])
        o_c = dilated_attention(c_q, c_k, c_v, cos, sin)
        o_d = sgu_mixer(d_u, d_v, sgu_ln_g[l], sgu_ln_b[l], sgu_w[l], sgu_b[l])
        mix = jnp.concatenate([o_a, o_b, o_c, o_d], axis=-1).astype(dt) @ w_out[l]
        x = x + g1 * mix

        h = rmsnorm(x, ln2_g[l]) * (1 + sc2) + sh2
        x = x + g2 * conv_ffn(h, ffn_up[l], ffn_conv_w[l], ffn_conv_b[l], ffn_down[l])

    return rmsnorm(x, lnf_g)
```

```python
import os
import math
import numpy as np
from contextlib import ExitStack
import concourse.bass as bass
import concourse.mybir as mybir
from concourse.bass_utils import run_bass_kernel_spmd

F32 = mybir.dt.float32
BF16 = mybir.dt.bfloat16
ALU = mybir.AluOpType
AF = mybir.ActivationFunctionType
AX = mybir.AxisListType

L = 4
S = 4096
D = 1024
TB = 512
NTB = S // TB
NIN = 2576
NINX = NIN + 512
DFF = 2816
EPS = 1e-6
PPL = 312
NPP = L * PPL + 8
NBC = 896
NCF = 1156
GELU_K = 2.0 * math.sqrt(2.0 / math.pi)


class Res:
    __slots__ = ("w", "r")

    def __init__(self):
        self.w = None
        self.r = []


class Op:
    __slots__ = ("eng", "fn", "deps", "dma", "sig", "sigval", "sem", "semval", "semprev", "phase", "persist", "seq")

    def __init__(self, eng, fn, dma, phase, persist):
        self.eng = eng
        self.fn = fn
        self.dma = dma
        self.deps = []
        self.sig = False
        self.sigval = 0
        self.sem = None
        self.semval = 0
        self.semprev = 0
        self.phase = phase
        self.persist = persist
        self.seq = 0


class Tile:
    __slots__ = ("t", "res")

    def __init__(self, t):
        self.t = t
        self.res = Res()

    def __getitem__(self, k):
        return self.t[k]


class Prog:
    ENGS = ("pe", "act", "dve", "pool", "sp")
    CENGS = ("pe", "act", "dve", "pool")

    def __init__(self, nc, stack, n_dma=(("sp", 24), ("pool", 12))):
        self.nc = nc
        self.phase = 0
        self.ops = []
        self.esets = [{e: stack.enter_context(nc.semaphore("s%d_%s" % (i, e))) for e in self.CENGS} for i in range(3)]
        self.seqn = 0
        self.dsem = {q: [stack.enter_context(nc.semaphore("d_%s%d" % (q, i))) for i in range(n)] for q, n in n_dma}
        self.cnt = {e: 0 for e in self.CENGS}
        self.dcnt = {q: 0 for q in self.dsem}
        self.dval = {q: [0] * len(self.dsem[q]) for q in self.dsem}
        self.pending_persist = []
        self.total_ops = 0

    def op(self, eng, fn, reads=(), writes=(), dma=False, persist=False):
        o = Op(eng, fn, dma, self.phase, persist)
        self.seqn += 1
        o.seq = self.seqn
        deps = {}
        for r in reads:
            if r.w is not None:
                deps[id(r.w)] = r.w
        for w in writes:
            if w.w is not None:
                deps[id(w.w)] = w.w
            for q in w.r:
                deps[id(q)] = q
        for r in reads:
            if not dma:
                r.r = [q for q in r.r if q.dma or q.eng != eng]
            r.r.append(o)
        for w in writes:
            w.w = o
            w.r = []
        dl = []
        best = {}
        for d in deps.values():
            if d.phase != self.phase:
                if d.dma and d.persist:
                    dl.append(d)
                continue
            if d.dma:
                dl.append(d)
                continue
            if d.eng == "pe" and eng == "pe" and not dma:
                continue
            b = best.get(d.eng)
            if b is None or d.seq > b.seq:
                best[d.eng] = d
        dl.extend(best.values())
        o.deps = dl
        self.ops.append(o)
        return o

    def end_phase(self):
        nc = self.nc
        ops = self.ops
        self.total_ops += len(ops)
        self.esem = self.esets[self.phase % 3]
        nxt = self.esets[(self.phase + 1) % 3]
        self.cnt = {e: 0 for e in self.CENGS}
        for o in ops:
            for d in o.deps:
                if not d.dma:
                    d.sig = True
        streams = {e: [] for e in self.ENGS}
        for o in ops:
            streams[o.eng].append(o)
        last = {}
        for e in self.CENGS:
            for o in reversed(streams[e]):
                if not o.dma:
                    o.sig = True
                    last[e] = o
                    break
        for o in ops:
            if o.dma:
                q = o.eng
                i = self.dcnt[q] % len(self.dsem[q])
                self.dcnt[q] += 1
                o.sem = self.dsem[q][i]
                o.semprev = self.dval[q][i]
                self.dval[q][i] += 16
                o.semval = self.dval[q][i]
            elif o.sig:
                self.cnt[o.eng] += 1
                o.sigval = self.cnt[o.eng]
        bar = {}
        for o in ops:
            if o.dma and not o.persist:
                bar[id(o.sem)] = (o.sem, o.semval)
        for e, o in last.items():
            bar[id(self.esem[e])] = (self.esem[e], o.sigval)
        esem = self.esem

        def run(ename, eng):
            waited = {}
            if ename in nxt:
                eng.sem_clear(nxt[ename])
            for o in streams[ename]:
                if o.dma and o.semprev > 0:
                    k = id(o.sem)
                    if waited.get(k, 0) < o.semprev:
                        eng.wait_ge(o.sem, o.semprev)
                        waited[k] = o.semprev
                for d in o.deps:
                    if d.dma:
                        s, v = d.sem, d.semval
                    else:
                        s, v = esem[d.eng], d.sigval
                    k = id(s)
                    if waited.get(k, 0) < v:
                        eng.wait_ge(s, v)
                        waited[k] = v
                ins = o.fn(eng)
                if o.dma:
                    ins.then_inc(o.sem, 16)
                elif o.sig:
                    ins.then_inc(esem[ename], 1)
            for s, v in bar.values():
                if waited.get(id(s), 0) < v:
                    eng.wait_ge(s, v)

        with nc.Block() as block:
            @block.tensor
            def _(eng):
                run("pe", eng)

            @block.scalar
            def _(eng):
                run("act", eng)

            @block.vector
            def _(eng):
                run("dve", eng)

            @block.gpsimd
            def _(eng):
                run("pool", eng)

            @block.sync
            def _(eng):
                run("sp", eng)
        self.maxcnt = max(getattr(self, 'maxcnt', 0), max(self.cnt.values()))
        assert max(self.cnt.values()) < 3000, self.cnt
        self.ops = []
        self.phase += 1

    def final_wait_persist(self):
        return [(o.sem, o.semval) for o in self.pending_persist]


def _pack_pp(inp):
    pp = np.zeros((128, NPP), np.float32)
    for l in range(L):
        o = l * PPL
        pp[:, o + 0:o + 48] = inp["ada_b"][l].reshape(48, 128).T
        pp[:, o + 48:o + 56] = inp["ln1_g"][l].reshape(8, 128).T
        pp[:, o + 56:o + 64] = inp["ln2_g"][l].reshape(8, 128).T
        pp[:, o + 64:o + 126] = inp["conv_w"][l].reshape(31, 2, 128).transpose(2, 1, 0).reshape(128, 62)
        pp[:, o + 126:o + 128] = inp["conv_b"][l].reshape(2, 128).T
        pp[:, o + 128:o + 130] = inp["cln_g"][l].reshape(2, 128).T
        pp[:, o + 130:o + 132] = inp["cln_b"][l].reshape(2, 128).T
        pp[:, o + 132:o + 136] = inp["sgu_b"][l].T
        pp[:, o + 136:o + 268] = inp["ffn_conv_w"][l].reshape(3, 44, 128).transpose(2, 1, 0).reshape(128, 132)
        pp[:, o + 268:o + 312] = inp["ffn_conv_b"][l].reshape(44, 128).T
    pp[:, L * PPL:L * PPL + 8] = inp["lnf_g"].reshape(8, 128).T
    return pp


def _pack_bc(inp):
    bc = np.zeros((L, 128, NBC), np.float32)
    for l in range(L):
        row = np.concatenate([inp["gla_b"][l], np.tile(inp["gla_norm_g"][l], 4), inp["sgu_ln_g"][l], inp["sgu_ln_b"][l]])
        bc[l] = np.broadcast_to(row[None, :], (128, NBC))
    return bc


def _consts():
    cf = np.zeros((128, NCF), np.float32)
    i = np.arange(128)
    cf[:, 0:128] = np.eye(128)
    cf[:, 128:256] = (i[:, None] <= i[None, :]) / 16.0
    cf[:, 256:384] = (i[:, None] > i[None, :]) / 16.0
    cf[:, 384:512] = (i[:, None] <= i[None, :])
    cf[:, 512:640] = (i[:, None] >= i[None, :])
    cf[:, 640:896] = ((i[:, None] // 32) == (np.arange(256)[None, :] // 64))
    cf[:, 896:1024] = ((i[None, :] < 64) & (i[:, None] == i[None, :] + 64))
    cf[:, 1024:1152] = ((i[None, :] >= 64) & (i[:, None] == i[None, :] - 64))
    cf[:, 1152:1156] = ((i[:, None] // 32) == np.arange(4)[None, :])
    inv = (1.0 / (np.float32(10000.0) ** (np.arange(0, 64, 2, dtype=np.float32) / np.float32(64)))).astype(np.float32)
    ang = np.arange(S, dtype=np.float32)[:, None] * inv[None, :]
    cs = np.zeros((2, 128, S), np.float32)
    f = i % 32
    cs[0] = np.cos(ang).astype(np.float32).T[f]
    cs[1] = np.sin(ang).astype(np.float32).T[f]
    return cf, cs


def build_program(nlayers=L, debug=False, stop_after=None, p3test=None, l0=0, final=True):
    nc = bass.Bass("TRN2", target_bir_lowering=False)
    EI = "ExternalInput"
    xT_in = nc.dram_tensor("xT", [D, S], F32, kind=EI).ap()
    cT_in = nc.dram_tensor("cT", [128, 8], F32, kind=EI).ap()
    ada_w = nc.dram_tensor("ada_w", [L, D, 6 * D], F32, kind=EI).ap()
    w_in = nc.dram_tensor("w_in", [L, D, NIN], F32, kind=EI).ap()
    w_out = nc.dram_tensor("w_out", [L, D, D], F32, kind=EI).ap()
    ffn_up = nc.dram_tensor("ffn_up", [L, D, 2 * DFF], F32, kind=EI).ap()
    ffn_down = nc.dram_tensor("ffn_down", [L, DFF, D], F32, kind=EI).ap()
    gla_w2 = nc.dram_tensor("gla_w2", [L, 16, 128], F32, kind=EI).ap()
    sgu_w = nc.dram_tensor("sgu_w", [L, 4, 128, 128], F32, kind=EI).ap()
    pp_in = nc.dram_tensor("pp", [128, NPP], F32, kind=EI).ap()
    bc_in = nc.dram_tensor("bc", [L, 128, NBC], F32, kind=EI).ap()
    cf_in = nc.dram_tensor("cf", [128, NCF], F32, kind=EI).ap()
    cs_in = nc.dram_tensor("cs", [2, 128, S], F32, kind=EI).ap()
    outT = nc.dram_tensor("outT", [D, S], F32, kind="ExternalOutput").ap() if final else None
    SK = "ExternalOutput" if debug else "Internal"
    xT_d = nc.dram_tensor("xT_d", [D, S], F32, kind=(SK if final else "ExternalOutput")).ap()
    oT_d = nc.dram_tensor("oT_d", [D, S], BF16, kind=SK).ap()
    qkv_d = nc.dram_tensor("qkv_d", [3, 256, S], BF16, kind=(EI if p3test else SK)).ap()
    h2T_d = nc.dram_tensor("h2T_d", [D, S], BF16, kind=SK).ap()

    def cp(ap):
        return ap.rearrange("(c p) t -> p c t", p=128)

    with ExitStack() as st:
        P = Prog(nc, st)

        uid = [0]

        def sbt(stack, name, shape, dt):
            uid[0] += 1
            return Tile(stack.enter_context(nc.sbuf_tensor("sb%d_%s" % (uid[0], name), shape, dt)))

        def pst(stack, name, shape, dt):
            return Tile(stack.enter_context(nc.psum_tensor("ps_" + name, shape, dt)))

        cf = sbt(st, "cf", [128, NCF], F32)
        pp = sbt(st, "pp", [128, NPP], F32)
        modt = sbt(st, "mod", [128, L, 48], F32)
        gm = sbt(st, "gm", [128, L, 16], F32)
        identb = sbt(st, "identb", [128, 128], BF16)
        onesb = sbt(st, "onesb", [128, 128], BF16)
        onesf = sbt(st, "onesf", [128, 128], F32)
        maskCb = sbt(st, "maskCb", [128, 256], BF16)
        maskLE4 = sbt(st, "maskLE4", [128, 512], F32)
        ws = sbt(st, "ws", [128, 34560], BF16)
        psf = [pst(st, "psf%d" % i, [128, 512], F32) for i in range(6)]
        psb = [pst(st, "psb%d" % i, [128, 1024], BF16) for i in range(2)]
        psi = [0, 0]

        def PS():
            t = psf[psi[0] % len(psf)]
            psi[0] += 1
            return t

        def PSB():
            t = psb[psi[1] % len(psb)]
            psi[1] += 1
            return t

        ws_in = ws[:, 0:8 * NINX].rearrange("p (k n) -> p k n", k=8)
        ws_out = ws[:, 8 * NINX:8 * NINX + 8192].rearrange("p (k n) -> p k n", k=8)
        ws_up = ws[:, 0:22528].rearrange("p (k n) -> p k n", k=8)
        ws_dn = ws[:, 22528:33792].rearrange("p (j n) -> p j n", j=11)

        ident = cf[:, 0:128]
        triT = cf[:, 128:256]
        triS = cf[:, 256:384]
        maskLE = cf[:, 384:512]
        blockmask = cf[:, 640:896]
        swapLo = cf[:, 896:1024]
        swapHi = cf[:, 1024:1152]
        headmask = cf[:, 1152:1156]

        wsr = [Res() for _ in range(40)]
        wsrot = Res()
        wsall = wsr + [wsrot]
        xres = [Res() for _ in range(NTB)]
        ores = {}
        qkvres = {}
        h2res = [Res() for _ in range(NTB)]

        def R(dct, key):
            if key not in dct:
                dct[key] = Res()
            return dct[key]

        def tcols(tb):
            return slice(tb * TB, (tb + 1) * TB)

        def act(out, in_, func, reads, writes, scale=1.0, bias=0.0):
            P.op("act", lambda e: e.activation(out=out, in_=in_, func=func, scale=scale, bias=bias), reads=reads, writes=writes)

        def dma(q, out, in_, reads=(), writes=(), persist=False):
            P.op(q, lambda e: e.dma_start(out=out, in_=in_), reads=reads, writes=writes, dma=True, persist=persist)

        def mm(out, lhsT, rhs, start, stop, reads, writes):
            P.op("pe", lambda e: e.matmul(out, lhsT=lhsT, rhs=rhs, start=start, stop=stop), reads=reads, writes=writes)

        def tt(eng, out, in0, in1, op, reads, writes):
            P.op(eng, lambda e: e.tensor_tensor(out=out, in0=in0, in1=in1, op=op), reads=reads, writes=writes)

        def ts(eng, out, in0, s1, s2, op0, op1, reads, writes):
            P.op(eng, lambda e: e.tensor_scalar(out=out, in0=in0, scalar1=s1, scalar2=s2, op0=op0, op1=op1), reads=reads, writes=writes)

        def stt(out, in0, scalar, in1, op0, op1, reads, writes):
            P.op("dve", lambda e: e.scalar_tensor_tensor(out=out, in0=in0, scalar=scalar, in1=in1, op0=op0, op1=op1), reads=reads, writes=writes)

        def recip(out, in_, reads, writes):
            P.op("dve", lambda e: e.reciprocal(out=out, in_=in_), reads=reads, writes=writes)

        def cpy(eng, out, in_, reads, writes):
            if eng == "act":
                act(out, in_, AF.Copy, reads, writes)
            else:
                P.op(eng, lambda e: e.tensor_copy(out=out, in_=in_), reads=reads, writes=writes)

        def memset(eng, ap, val, writes):
            P.op(eng, lambda e: e.memset(ap, val), writes=writes)

        def rmsnorm_fm(xt, gmcol, shcol, outT_tile, out_dt_f32, sq, f1, f2, f3):
            pssum = PS()
            for c in range(8):
                act(sq[:, :], xt[:, c, :], AF.Square, [xt.res], [sq.res])
                mm(pssum[:, :], onesb[:, :], sq[:, :], c == 0, c == 7, [onesb.res, sq.res], [pssum.res])
            act(f1[:, :], pssum[:, :], AF.Ln, [pssum.res], [f1.res], scale=1.0 / D, bias=EPS)
            act(f2[:, :], f1[:, :], AF.Exp, [f1.res], [f2.res], scale=-0.5)
            for c in range(8):
                if shcol is None:
                    stt(outT_tile[:, c, :], xt[:, c, :], gmcol[:, c:c + 1], f2[:, :], ALU.mult, ALU.mult,
                        [xt.res, f2.res, pp.res, gm.res], [outT_tile.res])
                else:
                    stt(f3[:, :], xt[:, c, :], gmcol[:, c:c + 1], f2[:, :], ALU.mult, ALU.mult,
                        [xt.res, f2.res, pp.res, gm.res], [f3.res])
                    ts("pool", outT_tile[:, c, :], f3[:, :], shcol[:, c:c + 1], 1.0, ALU.add, ALU.mult,
                       [f3.res, modt.res], [outT_tile.res])

        with ExitStack() as ph:
            c_sb = sbt(ph, "c_sb", [128, 8], F32)
            t8a = sbt(ph, "t8a", [128, 8], F32)
            t8b = sbt(ph, "t8b", [128, 8], F32)
            cond = sbt(ph, "cond", [128, 8], F32)
            aw = [sbt(ph, "aw%d" % i, [128, 8, 256], F32) for i in range(2)]
            modrow = sbt(ph, "modrow", [1, 6 * D], F32)
            dma("sp", cf[:, :], cf_in, writes=[cf.res])
            dma("sp", pp[:, :], pp_in, writes=[pp.res])
            dma("sp", c_sb[:, :], cT_in, writes=[c_sb.res])
            cpy("dve", identb[:, :], ident, [cf.res], [identb.res])
            memset("dve", onesb[:, :], 1.0, [onesb.res])
            memset("pool", onesf[:, :], 1.0, [onesf.res])
            cpy("dve", maskCb[:, 0:128], cf[:, 512:640], [cf.res], [maskCb.res])
            cpy("dve", maskCb[:, 128:256], cf[:, 384:512], [cf.res], [maskCb.res])
            for h in range(4):
                cpy("pool", maskLE4[:, h * 128:(h + 1) * 128], maskLE, [cf.res], [maskLE4.res])
            act(t8a[:, :], c_sb[:, :], AF.Exp, [c_sb.res], [t8a.res], scale=-1.0)
            ts("dve", t8a[:, :], t8a[:, :], 1.0, None, ALU.add, ALU.bypass, [t8a.res], [t8a.res])
            recip(t8b[:, :], t8a[:, :], [t8a.res], [t8b.res])
            tt("dve", cond[:, :], c_sb[:, :], t8b[:, :], ALU.mult, [c_sb.res, t8b.res], [cond.res])
            for l in (range(0) if p3test else range(l0, l0 + nlayers)):
                for n in range(24):
                    a = aw[n % 2]
                    dma("sp", a[:, :, :], ada_w[l][:, n * 256:(n + 1) * 256].rearrange("(k p) n -> p k n", p=128), writes=[a.res])
                    pr = PS()
                    for k in range(8):
                        mm(pr[0:1, 0:256], cond[:, k:k + 1], a[:, k, :], k == 0, k == 7, [cond.res, a.res], [pr.res])
                    cpy("act", modrow[0:1, n * 256:(n + 1) * 256], pr[0:1, 0:256], [pr.res], [modrow.res])
                pm = PS()
                for m in range(48):
                    mm(pm[:, m:m + 1], modrow[0:1, m * 128:(m + 1) * 128], cf[0:1, 0:1], True, True, [modrow.res, cf.res], [pm.res])
                o = l * PPL
                tt("dve", modt[:, l, :], pm[:, 0:48], pp[:, o:o + 48], ALU.add, [pm.res, pp.res], [modt.res])
                stt(gm[:, l, 0:8], modt[:, l, 8:16], 1.0, pp[:, o + 48:o + 56], ALU.add, ALU.mult, [modt.res, pp.res], [gm.res])
                stt(gm[:, l, 8:16], modt[:, l, 32:40], 1.0, pp[:, o + 56:o + 64], ALU.add, ALU.mult, [modt.res, pp.res], [gm.res])
            P.end_phase()

        for l in range(l0, l0 + nlayers):
            o = l * PPL
            xsrc = xT_in if l == l0 else xT_d
            sh1 = modt[:, l, 0:8]
            g1 = modt[:, l, 16:24]
            sh2 = modt[:, l, 24:32]
            g2 = modt[:, l, 40:48]
            with ExitStack() as ph:
              if not p3test:
                diag = sbt(ph, "diag", [128, 2, 31, 128], BF16)
                xt = sbt(ph, "xt", [128, 8, TB], F32)
                hTs = [sbt(ph, "hT%d" % i, [128, 8, TB], BF16) for i in range(2)]
                hA = [sbt(ph, "hA%d" % i, [128, 2, TB + 30], BF16) for i in range(2)]
                bcs = sbt(ph, "bcs", [128, NBC], F32)
                w2s = sbt(ph, "w2s", [16, 128], F32)
                sgWT = sbt(ph, "sgWT", [128, 4, 128], BF16)
                Sfull = sbt(ph, "Sfull", [128, 256], F32)
                Sm = [sbt(ph, "Sm%d" % i, [128, 256], BF16) for i in range(2)]
                sq = sbt(ph, "sq", [128, TB], BF16)
                lf = [sbt(ph, "lf%d" % i, [128, TB], F32) for i in range(3)]
                a_e = sbt(ph, "a_e", [128, TB], F32)
                yA = [sbt(ph, "yA%d" % i, [128, TB], F32) for i in range(2)]
                a_yb = sbt(ph, "a_yb", [128, TB], BF16)
                a_sb = sbt(ph, "a_sb", [128, TB], BF16)
                a_m = sbt(ph, "a_m", [128, TB], F32)
                a_m2 = sbt(ph, "a_m2", [128, TB], F32)
                a_rs = sbt(ph, "a_rs", [128, TB], F32)
                a_d = sbt(ph, "a_d", [128, TB], F32)
                a_o = sbt(ph, "a_o", [128, TB], BF16)
                lrs = sbt(ph, "lrs", [16, TB], F32)
                qbs = sbt(ph, "qbs", [128, TB], F32)
                kbs = sbt(ph, "kbs", [128, TB], F32)
                b_e1 = sbt(ph, "b_e1", [128, 128], F32)
                b_sp = sbt(ph, "b_sp", [128, 128], F32)
                b_E2 = [sbt(ph, "b_E%d" % i, [128, 384], F32) for i in range(2)]
                b_qt2 = [sbt(ph, "b_qt%d" % i, [128, 128], BF16) for i in range(2)]
                b_kt = sbt(ph, "b_kt", [128, 128], BF16)
                b_k42 = [sbt(ph, "b_k4%d" % i, [128, 512], BF16) for i in range(2)]
                b_ke2 = [sbt(ph, "b_ke%d" % i, [128, 128], BF16) for i in range(2)]
                b_kf = sbt(ph, "b_kf", [128, 128], F32)
                b_v2 = [sbt(ph, "b_v%d" % i, [128, 256], BF16) for i in range(2)]
                b_at = sbt(ph, "b_at", [128, 512], BF16)
                b_osq = sbt(ph, "b_osq", [128, 256], F32)
                b_s4 = sbt(ph, "b_s4", [128, 12], F32)
                b_eg = sbt(ph, "b_eg", [128, 256], F32)
                b_gt = sbt(ph, "b_gt", [128, 256], F32)
                b_ob = sbt(ph, "b_ob", [128, 256], BF16)
                oBT = sbt(ph, "oBT", [128, 2, TB], BF16)
                d_xs = sbt(ph, "d_xs", [128, 512], F32)
                d_a = sbt(ph, "d_a", [128, 512], F32)
                d_b = sbt(ph, "d_b", [128, 512], F32)
                d_ge = sbt(ph, "d_ge", [128, 512], F32)
                d_st = sbt(ph, "d_st", [128, 16], F32)
                d_vn = sbt(ph, "d_vn", [128, 256], F32)
                d_vl = sbt(ph, "d_vl", [128, 256], BF16)
                d_od = sbt(ph, "d_od", [128, 256], BF16)
                oDT = sbt(ph, "oDT", [128, 2, TB], BF16)
                csc = sbt(ph, "csc", [128, TB], F32)
                css = sbt(ph, "css", [128, TB], F32)
                c_t1 = sbt(ph, "c_t1", [128, TB], F32)
                c_t2 = sbt(ph, "c_t2", [128, TB], F32)
                c_o = [sbt(ph, "c_o%d" % i, [128, TB], BF16) for i in range(2)]

                for k in range(8):
                    dma("pool", ws_in[:, k, 0:NIN], w_in[l][k * 128:(k + 1) * 128, :], writes=[wsr[k]])
                dma("sp", bcs[:, :], bc_in[l], writes=[bcs.res])
                dma("sp", w2s[:, :], gla_w2[l], writes=[w2s.res])
                dma("sp", d_xs[:, :].rearrange("p (g s) -> p g s", g=4), sgu_w[l].rearrange("g t s -> t g s"), writes=[d_xs.res])
                for qi, c0 in enumerate((1296, 1552)):
                    r0 = NIN + qi * 256
                    for k in range(8):
                        src = ws_in[:, k, c0:c0 + 256].rearrange("p (h t r) -> p h t r", h=4, t=2)
                        dst = ws_in[:, k, r0:r0 + 256].rearrange("p (h t r) -> p h t r", h=4, t=2)
                        ts("dve" if k % 2 == 0 else "pool", dst[:, :, 0, :], src[:, :, 1, :], -1.0, 1.0, ALU.mult, ALU.mult, [wsr[k]], [wsrot])
                        cpy("pool" if k % 2 == 0 else "dve", dst[:, :, 1, :], src[:, :, 0, :], [wsr[k]], [wsrot])
                for c in range(2):
                    for k in range(31):
                        ts("dve" if (k % 2 == 0) else "pool", diag[:, c, k, :], ident, pp[:, o + 64 + c * 31 + k:o + 64 + c * 31 + k + 1], 1.0,
                           ALU.mult, ALU.mult, [cf.res, pp.res], [diag.res])
                psg = PS()
                for g in range(4):
                    P.op("pe", lambda e, g=g: e.transpose(out=psg[:, g * 128:(g + 1) * 128], in_=d_xs[:, g * 128:(g + 1) * 128], identity=ident),
                         reads=[d_xs.res, cf.res], writes=[psg.res])
                tt("dve", sgWT[:, :, :].rearrange("p g t -> p (g t)"), psg[:, :], maskLE4[:, :], ALU.mult, [psg.res, maskLE4.res], [sgWT.res])
                memset("dve", Sfull[:, :], 0.0, [Sfull.res])
                memset("pool", Sm[0][:, :], 0.0, [Sm[0].res])
                memset("pool", hA[0][:, :, 0:30], 0.0, [hA[0].res])
                memset("pool", hA[1][:, :, 0:30], 0.0, [hA[1].res])

                def fm(hT, c0, M=128):
                    pt = PS()
                    for k in range(8):
                        mm(pt[0:M, :], ws_in[:, k, c0:c0 + M], hT[:, k, :], k == 0, k == 7, wsall + [hT.res], [pt.res])
                    return pt

                def tm(hT, tt_, c0, N):
                    pt = PS()
                    for k in range(8):
                        mm(pt[:, 0:N], hT[:, k, tt_ * 128:(tt_ + 1) * 128], ws_in[:, k, c0:c0 + N], k == 0, k == 7, wsall + [hT.res], [pt.res])
                    return pt

                schunk = [0]

                def sigm(t, src, reads):
                    act(t[:, :], src, AF.Exp, reads, [t.res], scale=-1.0)
                    act(t[:, :], t[:, :], AF.Ln, [t.res], [t.res], bias=1.0)
                    act(t[:, :], t[:, :], AF.Exp, [t.res], [t.res], scale=-1.0)

                def genA(tb, tc):
                    hT = hTs[tb % 2]
                    hcur = hA[tb % 2]
                    hprev = hA[(tb + 1) % 2]
                    if tb > 0:
                        cpy("pool", hcur[:, :, 0:30], hprev[:, :, TB:TB + 30], [hprev.res], [hcur.res])
                    for c in range(2):
                        pv = fm(hT, c * 128)
                        pg = fm(hT, 256 + c * 128)
                        sigm(a_e, pg[:, :], [pg.res])
                        tt("dve", hcur[:, c, 30:30 + TB], pv[:, :], a_e[:, :], ALU.mult, [pv.res, a_e.res], [hcur.res])
                        yield
                        py = PS()
                        for k in range(31):
                            mm(py[:, :], diag[:, c, k, :], hcur[:, c, k:k + TB], k == 0, k == 30, [diag.res, hcur.res], [py.res])
                        act(yA[c][:, :], py[:, :], AF.Identity, [py.res, pp.res], [yA[c].res], bias=pp[:, o + 126 + c:o + 127 + c])
                        yield
                    pmean = PS()
                    pmsq = PS()
                    for c in range(2):
                        cpy("dve", a_yb[:, :], yA[c][:, :], [yA[c].res], [a_yb.res])
                        act(a_sb[:, :], yA[c][:, :], AF.Square, [yA[c].res], [a_sb.res])
                        mm(pmean[:, :], onesb[:, :], a_yb[:, :], c == 0, c == 1, [onesb.res, a_yb.res], [pmean.res])
                        mm(pmsq[:, :], onesb[:, :], a_sb[:, :], c == 0, c == 1, [onesb.res, a_sb.res], [pmsq.res])
                    act(a_m[:, :], pmean[:, :], AF.Copy, [pmean.res], [a_m.res], scale=1.0 / 256)
                    tt("dve", a_m2[:, :], a_m[:, :], a_m[:, :], ALU.mult, [a_m.res], [a_m2.res])
                    stt(a_m2[:, :], pmsq[:, :], 1.0 / 256, a_m2[:, :], ALU.mult, ALU.subtract, [pmsq.res, a_m2.res], [a_m2.res])
                    act(a_rs[:, :], a_m2[:, :], AF.Ln, [a_m2.res], [a_rs.res], bias=EPS)
                    act(a_rs[:, :], a_rs[:, :], AF.Exp, [a_rs.res], [a_rs.res], scale=-0.5)
                    yield
                    for c in range(2):
                        tt("dve", a_d[:, :], yA[c][:, :], a_m[:, :], ALU.subtract, [yA[c].res, a_m.res], [a_d.res])
                        tt("dve", a_d[:, :], a_d[:, :], a_rs[:, :], ALU.mult, [a_d.res, a_rs.res], [a_d.res])
                        act(a_d[:, :], a_d[:, :], AF.Identity, [a_d.res, pp.res], [a_d.res],
                            scale=pp[:, o + 128 + c:o + 129 + c], bias=pp[:, o + 130 + c:o + 131 + c])
                        sigm(a_e, a_d[:, :], [a_d.res])
                        tt("dve", a_o[:, :], a_d[:, :], a_e[:, :], ALU.mult, [a_d.res, a_e.res], [a_o.res])
                        dma("sp", oT_d[c * 128:(c + 1) * 128, tc], a_o[:, :], reads=[a_o.res], writes=[R(ores, (c, tb))])
                        yield

                bfront_done = {}
                bback_done = {}

                def genB(tb, tc):
                    hT = hTs[tb % 2]
                    pq = fm(hT, 512)
                    cpy("act", qbs[:, :], pq[:, :], [pq.res], [qbs.res])
                    pk = fm(hT, 640)
                    cpy("act", kbs[:, :], pk[:, :], [pk.res], [kbs.res])
                    plr = fm(hT, 1280, 16)
                    cpy("act", lrs[:, :], plr[0:16, :], [plr.res], [lrs.res])
                    yield
                    for t4 in range(4):
                        t0 = t4 * 128
                        tsl = slice(t0, t0 + 128)
                        i2 = t4 % 2
                        gidx = tb * 4 + t4
                        while gidx >= 2 and (gidx - 2) not in bback_done:
                            yield
                        pkv = tm(hT, t4, 640, 384)
                        cpy("act", b_v2[i2][:, :], pkv[:, 128:384], [pkv.res], [b_v2[i2].res])
                        cpy("act", b_kf[:, :], pkv[:, 0:128], [pkv.res], [b_kf.res])
                        pgk = PS()
                        mm(pgk[:, 0:128], lrs[0:16, tsl], w2s[0:16, :], True, False, [lrs.res, w2s.res], [pgk.res])
                        mm(pgk[:, 0:128], onesf[0:1, 0:128], bcs[0:1, 0:128], False, True, [onesf.res, bcs.res], [pgk.res])
                        act(b_e1[:, :], pgk[:, 0:128], AF.Exp, [pgk.res], [b_e1.res], scale=-1.0)
                        act(b_sp[:, :], b_e1[:, :], AF.Ln, [b_e1.res], [b_sp.res], bias=1.0)
                        yield
                        bE = b_E2[i2]
                        pB = PS()
                        mm(pB[:, 0:128], b_sp[:, :], triT, True, True, [b_sp.res, cf.res], [pB.res])
                        mm(pB[:, 128:256], triS, b_sp[:, :], True, True, [b_sp.res, cf.res], [pB.res])
                        act(bE[:, 0:128], pB[:, 0:128], AF.Exp, [pB.res], [bE.res], scale=-1.0)
                        act(bE[:, 128:256], pB[:, 0:128], AF.Exp, [pB.res], [bE.res], scale=1.0)
                        act(bE[:, 256:384], pB[:, 128:256], AF.Exp, [pB.res], [bE.res], scale=-1.0)
                        yield
                        stt(b_qt2[i2][:, :], qbs[:, tsl], 32.0 ** -0.5, bE[:, 0:128], ALU.mult, ALU.mult, [qbs.res, bE.res], [b_qt2[i2].res])
                        tt("dve", b_kt[:, :], kbs[:, tsl], bE[:, 128:256], ALU.mult, [kbs.res, bE.res], [b_kt.res])
                        for h in range(4):
                            ts("pool", b_k42[i2][:, h * 128:(h + 1) * 128], b_kt[:, :], headmask[:, h:h + 1], 1.0, ALU.mult, ALU.mult,
                               [b_kt.res, cf.res], [b_k42[i2].res])
                        tt("dve", b_ke2[i2][:, :], b_kf[:, :], bE[:, 256:384], ALU.mult, [b_kf.res, bE.res], [b_ke2[i2].res])
                        bfront_done[(tb, t4)] = True
                        yield

                def genBb(tb, tc):
                    hT = hTs[tb % 2]
                    for t4 in range(4):
                        t0 = t4 * 128
                        tsl = slice(t0, t0 + 128)
                        i2 = t4 % 2
                        while (tb, t4) not in bfront_done:
                            yield
                        bE = b_E2[i2]
                        b_qt = b_qt2[i2]
                        b_k4 = b_k42[i2]
                        b_ke = b_ke2[i2]
                        b_v = b_v2[i2]
                        patt = PS()
                        for h in range(4):
                            mm(patt[:, h * 128:(h + 1) * 128], b_k4[:, h * 128:(h + 1) * 128], b_qt[:, :], True, True,
                               [b_k4.res, b_qt.res], [patt.res])
                        tt("dve", b_at[:, :], patt[:, :], maskLE4[:, :], ALU.mult, [patt.res, maskLE4.res], [b_at.res])
                        yield
                        smp = Sm[schunk[0] % 2]
                        smn = Sm[(schunk[0] + 1) % 2]
                        schunk[0] += 1
                        po = PS()
                        mm(po[:, 0:256], b_qt[:, :], smp[:, :], True, False, [b_qt.res, smp.res], [po.res])
                        for h in range(4):
                            mm(po[:, h * 64:(h + 1) * 64], b_at[:, h * 128:(h + 1) * 128], b_v[:, h * 64:(h + 1) * 64], False, h == 3,
                               [b_at.res, b_v.res], [po.res])
                        pdS = PS()
                        mm(pdS[:, 0:256], b_ke[:, :], b_v[:, :], True, True, [b_ke.res, b_v.res], [pdS.res])
                        stt(Sfull[:, :], Sfull[:, :], bE[:, 127:128], pdS[:, 0:256], ALU.mult, ALU.add, [Sfull.res, bE.res, pdS.res], [Sfull.res])
                        tt("pool", smn[:, :], Sfull[:, :], blockmask, ALU.mult, [Sfull.res, cf.res], [smn.res])
                        pgt = tm(hT, t4, 1024, 256)
                        act(b_osq[:, :], po[:, 0:256], AF.Square, [po.res], [b_osq.res])
                        P.op("dve", lambda e: e.reduce_sum(out=b_s4[:, 0:4], in_=b_osq[:, :].rearrange("p (h e) -> p h e", h=4), axis=AX.X),
                             reads=[b_osq.res], writes=[b_s4.res])
                        act(b_s4[:, 4:8], b_s4[:, 0:4], AF.Ln, [b_s4.res], [b_s4.res], scale=1.0 / 64, bias=EPS)
                        act(b_s4[:, 8:12], b_s4[:, 4:8], AF.Exp, [b_s4.res], [b_s4.res], scale=-0.5)
                        sigm(b_eg, pgt[:, 0:256], [pgt.res])
                        tt("dve", b_gt[:, :], pgt[:, 0:256], b_eg[:, :], ALU.mult, [pgt.res, b_eg.res], [b_gt.res])
                        tt("pool", b_gt[:, :], b_gt[:, :], bcs[:, 128:384], ALU.mult, [b_gt.res, bcs.res], [b_gt.res])
                        for h in range(4):
                            stt(b_ob[:, h * 64:(h + 1) * 64], po[:, h * 64:(h + 1) * 64], b_s4[:, 8 + h:9 + h], b_gt[:, h * 64:(h + 1) * 64],
                                ALU.mult, ALU.mult, [po.res, b_s4.res, b_gt.res], [b_ob.res])
                        yield
                        yield
                        yield
                        yield
                        ptr = PSB()
                        for cc in range(2):
                            P.op("pe", lambda e, cc=cc, ptr=ptr: e.transpose(out=ptr[:, cc * 128:(cc + 1) * 128], in_=b_ob[:, cc * 128:(cc + 1) * 128], identity=identb[:, :]),
                                 reads=[b_ob.res, identb.res], writes=[ptr.res])
                        cpy("act", oBT[:, :, tsl], ptr[:, 0:256].rearrange("p (c t) -> p c t", c=2), [ptr.res], [oBT.res])
                        bback_done[tb * 4 + t4] = True
                        yield
                    dma("sp", cp(oT_d[256:512, :])[:, :, tc], oBT[:, :, :], reads=[oBT.res], writes=[R(ores, (2, tb)), R(ores, (3, tb))])

                def genD(tb, tc):
                    hT = hTs[tb % 2]
                    for t4 in range(4):
                        t0 = t4 * 128
                        tsl = slice(t0, t0 + 128)
                        puv = tm(hT, t4, 2064, 512)
                        cpy("act", d_xs[:, :], puv[:, :], [puv.res], [d_xs.res])
                        yield
                        tt("pool", d_a[:, :], d_xs[:, :], d_xs[:, :], ALU.mult, [d_xs.res], [d_a.res])
                        ts("pool", d_a[:, :], d_a[:, :], 0.044715, 1.0, ALU.mult, ALU.add, [d_a.res], [d_a.res])
                        tt("dve", d_a[:, :], d_a[:, :], d_xs[:, :], ALU.mult, [d_a.res, d_xs.res], [d_a.res])
                        act(d_b[:, :], d_a[:, :], AF.Exp, [d_a.res], [d_b.res], scale=-GELU_K)
                        act(d_b[:, :], d_b[:, :], AF.Ln, [d_b.res], [d_b.res], bias=1.0)
                        act(d_b[:, :], d_b[:, :], AF.Exp, [d_b.res], [d_b.res], scale=-1.0)
                        tt("dve", d_ge[:, :], d_xs[:, :], d_b[:, :], ALU.mult, [d_xs.res, d_b.res], [d_ge.res])
                        yield
                        P.op("dve", lambda e: e.bn_stats(out=d_st[:, 0:6], in_=d_ge[:, 256:512]), reads=[d_ge.res], writes=[d_st.res])
                        P.op("dve", lambda e: e.bn_aggr(out=d_st[:, 8:10], in_=d_st[:, 0:6]), reads=[d_st.res], writes=[d_st.res])
                        act(d_st[:, 10:11], d_st[:, 9:10], AF.Ln, [d_st.res], [d_st.res], bias=EPS)
                        act(d_st[:, 11:12], d_st[:, 10:11], AF.Exp, [d_st.res], [d_st.res], scale=-0.5)
                        ts("dve", d_vn[:, :], d_ge[:, 256:512], d_st[:, 8:9], d_st[:, 11:12], ALU.subtract, ALU.mult, [d_ge.res, d_st.res], [d_vn.res])
                        tt("dve", d_vn[:, :], d_vn[:, :], bcs[:, 384:640], ALU.mult, [d_vn.res, bcs.res], [d_vn.res])
                        tt("pool", d_vl[:, :], d_vn[:, :], bcs[:, 640:896], ALU.add, [d_vn.res, bcs.res], [d_vl.res])
                        yield
                        yield
                        yield
                        psv = PS()
                        for g in range(4):
                            mm(psv[:, g * 64:(g + 1) * 64], sgWT[:, g, :], d_vl[:, g * 64:(g + 1) * 64], True, True, [sgWT.res, d_vl.res], [psv.res])
                        for g in range(4):
                            stt(d_od[:, g * 64:(g + 1) * 64], psv[:, g * 64:(g + 1) * 64], pp[:, o + 132 + g:o + 133 + g], d_ge[:, g * 64:(g + 1) * 64],
                                ALU.add, ALU.mult, [psv.res, pp.res, d_ge.res], [d_od.res])
                        yield
                        yield
                        yield
                        ptr = PSB()
                        for cc in range(2):
                            P.op("pe", lambda e, cc=cc, ptr=ptr: e.transpose(out=ptr[:, cc * 128:(cc + 1) * 128], in_=d_od[:, cc * 128:(cc + 1) * 128], identity=identb[:, :]),
                                 reads=[d_od.res, identb.res], writes=[ptr.res])
                        cpy("act", oDT[:, :, tsl], ptr[:, 0:256].rearrange("p (c t) -> p c t", c=2), [ptr.res], [oDT.res])
                        yield
                    dma("sp", cp(oT_d[768:1024, :])[:, :, tc], oDT[:, :, :], reads=[oDT.res], writes=[R(ores, (6, tb)), R(ores, (7, tb))])

                def genC(tb, tc):
                    hT = hTs[tb % 2]
                    dma("sp", csc[:, :], cs_in[0][:, tc], writes=[csc.res])
                    dma("sp", css[:, :], cs_in[1][:, tc], writes=[css.res])
                    ci = 0
                    for qi, c0 in enumerate((1296, 1552)):
                        r0 = NIN + qi * 256
                        for c in range(2):
                            p1 = fm(hT, c0 + c * 128)
                            tt("dve", c_t1[:, :], p1[:, :], csc[:, :], ALU.mult, [p1.res, csc.res], [c_t1.res])
                            p2 = fm(hT, r0 + c * 128)
                            tt("dve", c_t2[:, :], p2[:, :], css[:, :], ALU.mult, [p2.res, css.res], [c_t2.res])
                            co = c_o[ci % 2]
                            ci += 1
                            tt("pool", co[:, :], c_t1[:, :], c_t2[:, :], ALU.add, [c_t1.res, c_t2.res], [co.res])
                            dma("sp", qkv_d[qi][c * 128:(c + 1) * 128, tc], co[:, :], reads=[co.res], writes=[R(qkvres, (qi, c))])
                            yield
                    for c in range(2):
                        p1 = fm(hT, 1808 + c * 128)
                        co = c_o[ci % 2]
                        ci += 1
                        cpy("act", co[:, :], p1[:, :], [p1.res], [co.res])
                        dma("sp", qkv_d[2][c * 128:(c + 1) * 128, tc], co[:, :], reads=[co.res], writes=[R(qkvres, (2, c))])
                        yield

                def genLN(tb, tc):
                    hT = hTs[tb % 2]
                    dma("sp", xt[:, :, :], cp(xsrc)[:, :, tc], reads=[xres[tb]], writes=[xt.res])
                    yield
                    pssum = PS()
                    for c in range(8):
                        act(sq[:, :], xt[:, c, :], AF.Square, [xt.res], [sq.res])
                        mm(pssum[:, :], onesb[:, :], sq[:, :], c == 0, c == 7, [onesb.res, sq.res], [pssum.res])
                    act(lf[0][:, :], pssum[:, :], AF.Ln, [pssum.res], [lf[0].res], scale=1.0 / D, bias=EPS)
                    act(lf[1][:, :], lf[0][:, :], AF.Exp, [lf[0].res], [lf[1].res], scale=-0.5)
                    yield
                    for c in range(8):
                        f3 = lf[0] if c % 2 == 0 else lf[2]
                        stt(f3[:, :], xt[:, c, :], gm[:, l, c:c + 1], lf[1][:, :], ALU.mult, ALU.mult, [xt.res, lf[1].res, gm.res], [f3.res])
                        ts("pool", hT[:, c, :], f3[:, :], sh1[:, c:c + 1], 1.0, ALU.add, ALU.mult, [f3.res, modt.res], [hT.res])
                        if c % 2 == 1:
                            yield

                makers = {"A": genA, "B": genB, "Bb": genBb, "D": genD, "C": genC}
                order = ("B", "Bb", "D", "A", "C", "LN")
                nxt_tb = {m: 0 for m in makers}
                active = {m: None for m in makers}
                ln_next = 0
                ln_done = -1
                ln_gen = None
                while True:
                    progressed = False
                    for m in makers:
                        if active[m] is None and nxt_tb[m] < NTB and nxt_tb[m] <= ln_done:
                            active[m] = makers[m](nxt_tb[m], tcols(nxt_tb[m]))
                    if ln_gen is None and ln_next < NTB and min(nxt_tb.values()) >= ln_next - 1:
                        ln_gen = genLN(ln_next, tcols(ln_next))
                    for m in order:
                        if m == "LN":
                            if ln_gen is not None:
                                progressed = True
                                try:
                                    next(ln_gen)
                                except StopIteration:
                                    ln_gen = None
                                    ln_done = ln_next
                                    ln_next += 1
                        elif active[m] is not None:
                            progressed = True
                            try:
                                next(active[m])
                            except StopIteration:
                                active[m] = None
                                nxt_tb[m] += 1
                    if not progressed and all(v >= NTB for v in nxt_tb.values()):
                        break
                    assert progressed or ln_gen is not None or any(nxt_tb[m] <= ln_done for m in makers if nxt_tb[m] < NTB), "scheduler stuck"
                P.end_phase()
            if stop_after == (l, 1):
                break

            with ExitStack() as ph:
                qT = sbt(ph, "qT", [128, S], BF16)
                kT = sbt(ph, "kT", [128, S], BF16)
                vT = sbt(ph, "vT", [128, S], BF16)
                acc = [sbt(ph, "acc%d" % i, [128, S], F32) for i in range(2)]
                vz = [sbt(ph, "vz%d" % i, [128, 256], BF16) for i in range(5)]
                Pm = [sbt(ph, "Pm%d" % i, [128, 256], BF16) for i in range(6)]
                rden = sbt(ph, "rden", [128, TB], F32)
                oC = [sbt(ph, "oC%d" % i, [128, TB], BF16) for i in range(2)]
                for z in vz:
                    memset("pool", z[:, :], 1.0, [z.res])
                pmi = 0
                for hp in (p3test["hps"] if p3test else range(2)):
                    dma("sp", qT[:, :], qkv_d[0][hp * 128:(hp + 1) * 128, :], reads=[R(qkvres, (0, hp))], writes=[qT.res])
                    dma("sp", kT[:, :], qkv_d[1][hp * 128:(hp + 1) * 128, :], reads=[R(qkvres, (1, hp))], writes=[kT.res])
                    dma("sp", vT[:, :], qkv_d[2][hp * 128:(hp + 1) * 128, :], reads=[R(qkvres, (2, hp))], writes=[vT.res])
                    vzi = 0
                    pend = []
                    for d in (p3test["branches"] if p3test else (1, 4, 16)):
                        nb = 32 // d
                        for r in range(d):
                            zprev = None
                            for n in range(nb):
                                cur = slice(r + d * 128 * n, r + d * 128 * n + d * 127 + 1, d)
                                prv = slice(r + d * 128 * (n - 1), r + d * 128 * (n - 1) + d * 127 + 1, d) if n > 0 else None
                                zc = vz[vzi % 5]
                                vzi += 1
                                ptr = PSB()
                                P.op("pe", lambda e, ptr=ptr, cur=cur: e.transpose(out=ptr[:, 0:128], in_=vT[:, cur], identity=identb[:, :]),
                                     reads=[vT.res, identb.res], writes=[ptr.res])
                                cpy("act", zc[:, 0:64], ptr[:, 0:64], [ptr.res], [zc.res])
                                cpy("act", zc[:, 192:256], ptr[:, 64:128], [ptr.res], [zc.res])
                                for hh in range(2):
                                    p0 = 64 * hh
                                    pS = PS()
                                    if n > 0:
                                        mm(pS[:, 0:128], kT[p0:p0 + 64, prv], qT[p0:p0 + 64, cur], True, True, [kT.res, qT.res], [pS.res])
                                    mm(pS[:, 128:256], kT[p0:p0 + 64, cur], qT[p0:p0 + 64, cur], True, True, [kT.res, qT.res], [pS.res])
                                    pm_ = Pm[pmi % len(Pm)]
                                    eng = "dve" if pmi % 2 == 0 else "pool"
                                    pmi += 1
                                    lo = 0 if n > 0 else 128
                                    act(pm_[:, lo:256], pS[:, lo:256], AF.Exp, [pS.res], [pm_.res], scale=0.125)
                                    tt(eng, pm_[:, lo:256], pm_[:, lo:256], maskCb[:, lo:256], ALU.mult, [pm_.res, maskCb.res], [pm_.res])

                                    def stage2(n=n, hh=hh, zprev=zprev, zc=zc, pm_=pm_, cur=cur, d=d):
                                        pO = PS()
                                        if n > 0:
                                            mm(pO[:, 0:128], zprev[:, hh * 128:(hh + 1) * 128], pm_[:, 0:128], True, False, [zprev.res, pm_.res], [pO.res])
                                        mm(pO[:, 0:128], zc[:, hh * 128:(hh + 1) * 128], pm_[:, 128:256], n == 0, True, [zc.res, pm_.res], [pO.res])
                                        if d == (p3test["branches"][0] if p3test else 1):
                                            cpy("dve", acc[hh][:, cur], pO[:, 0:128], [pO.res], [acc[hh].res])
                                        else:
                                            tt("dve", acc[hh][:, cur], acc[hh][:, cur], pO[:, 0:128], ALU.add, [acc[hh].res, pO.res], [acc[hh].res])
                                    pend.append(stage2)
                                    while len(pend) > 3:
                                        pend.pop(0)()
                                zprev = zc
                    while pend:
                        pend.pop(0)()
                    for tb in range(NTB):
                        tc = tcols(tb)
                        pden = PS()
                        mm(pden[:, :], swapLo, acc[0][:, tc], True, False, [cf.res, acc[0].res], [pden.res])
                        mm(pden[:, :], swapHi, acc[1][:, tc], False, True, [cf.res, acc[1].res], [pden.res])
                        recip(rden[:, :], pden[:, :], [pden.res], [rden.res])
                        oc = oC[tb % 2]
                        tt("dve", oc[0:64, :], acc[0][0:64, tc], rden[0:64, :], ALU.mult, [acc[0].res, rden.res], [oc.res])
                        tt("dve", oc[64:128, :], acc[1][64:128, tc], rden[64:128, :], ALU.mult, [acc[1].res, rden.res], [oc.res])
                        dma("sp", oT_d[512 + hp * 128:512 + (hp + 1) * 128, tc], oc[:, :], reads=[oc.res], writes=[R(ores, (4 + hp, tb))])
                P.end_phase()
            if stop_after == (l, 3):
                break

            for f in range(2):
                with ExitStack() as ph:
                    xts = [sbt(ph, "xt%d" % i, [128, 8, TB], F32) for i in range(2)]
                    h2Ts = [sbt(ph, "h2T%d" % i, [128, 8, TB], BF16) for i in range(2)]
                    gTs = [sbt(ph, "gT%d" % i, [128, 11, TB], BF16) for i in range(2)]
                    yb = [[sbt(ph, "yb%d_%d" % (h_, i), [128, TB + 2], F32) for i in range(2)] for h_ in range(2)]
                    halo = sbt(ph, "halo", [128, 22, 2], F32)
                    cc_ = [[sbt(ph, "cc%d_%d" % (h_, i), [128, TB], F32) for i in range(2)] for h_ in range(2)]
                    f_es = [sbt(ph, "f_e%d" % i, [128, TB], F32) for i in range(2)]
                    f_ss = [sbt(ph, "f_s%d" % i, [128, TB], F32) for i in range(2)]
                    if f == 0:
                        wo = sbt(ph, "wo", [128, 8, D], BF16)
                        oTt = sbt(ph, "oTt", [128, 8, TB], BF16)
                        sq = sbt(ph, "sq", [128, TB], BF16)
                        lf = [sbt(ph, "lf%d" % i, [128, TB], F32) for i in range(3)]
                        wor = [Res() for _ in range(8)]
                        for k in range(8):
                            dma("pool", wo[:, k, :], w_out[l][k * 128:(k + 1) * 128, :], writes=[wor[k]])
                    for k in range(8):
                        dma("pool", ws_up[:, k, 0:1408], ffn_up[l][k * 128:(k + 1) * 128, f * 1408:(f + 1) * 1408], writes=[wsr[2 * k]])
                        dma("pool", ws_up[:, k, 1408:2816], ffn_up[l][k * 128:(k + 1) * 128, DFF + f * 1408:DFF + (f + 1) * 1408], writes=[wsr[2 * k + 1]])
                    for jj in range(11):
                        dma("pool", ws_dn[:, jj, :], ffn_down[l][(f * 11 + jj) * 128:(f * 11 + jj + 1) * 128, :], writes=[wsr[16 + jj]])
                    memset("pool", halo[:, :, :], 0.0, [halo.res])

                    def stage_in(tb, part):
                        tc = tcols(tb)
                        xt = xts[tb % 2]
                        h2T = h2Ts[tb % 2]
                        if f == 1:
                            if part == 0:
                                dma("sp", h2T[:, :, :], cp(h2T_d)[:, :, tc], reads=[h2res[tb]], writes=[h2T.res])
                                dma("sp", xt[:, :, :], cp(xT_d)[:, :, tc], reads=[xres[tb]], writes=[xt.res])
                            return
                        if part == 0:
                            dma("sp", xt[:, :, :], cp(xsrc)[:, :, tc], reads=[xres[tb]], writes=[xt.res])
                            dma("sp", oTt[:, :, :], cp(oT_d)[:, :, tc], reads=[R(ores, (c, tb)) for c in range(8)], writes=[oTt.res])
                            for m in range(8):
                                pm = PS()
                                for kc in range(8):
                                    mm(pm[:, :], wo[:, kc, m * 128:(m + 1) * 128], oTt[:, kc, :], kc == 0, kc == 7, wor + [oTt.res], [pm.res])
                                stt(xt[:, m, :], pm[:, :], g1[:, m:m + 1], xt[:, m, :], ALU.mult, ALU.add, [pm.res, modt.res, xt.res], [xt.res])
                        elif part == 1:
                            pssum = PS()
                            for c in range(8):
                                act(sq[:, :], xt[:, c, :], AF.Square, [xt.res], [sq.res])
                                mm(pssum[:, :], onesb[:, :], sq[:, :], c == 0, c == 7, [onesb.res, sq.res], [pssum.res])
                            act(lf[0][:, :], pssum[:, :], AF.Ln, [pssum.res], [lf[0].res], scale=1.0 / D, bias=EPS)
                            act(lf[1][:, :], lf[0][:, :], AF.Exp, [lf[0].res], [lf[1].res], scale=-0.5)
                        else:
                            for c in range(8):
                                f3 = lf[0] if c % 2 == 0 else lf[2]
                                stt(f3[:, :], xt[:, c, :], gm[:, l, 8 + c:9 + c], lf[1][:, :], ALU.mult, ALU.mult, [xt.res, lf[1].res, gm.res], [f3.res])
                                ts("pool", h2T[:, c, :], f3[:, :], sh2[:, c:c + 1], 1.0, ALU.add, ALU.mult, [f3.res, modt.res], [h2T.res])
                            dma("sp", cp(h2T_d)[:, :, tc], h2T[:, :, :], reads=[h2T.res], writes=[h2res[tb]])

                    pend = []
                    pi = 0
                    for part in range(3):
                        stage_in(0, part)
                    for tb in range(NTB):
                        tc = tcols(tb)
                        xt = xts[tb % 2]
                        h2T = h2Ts[tb % 2]
                        gT = gTs[tb % 2]
                        for jj in range(11):
                            cvs = []
                            for half in range(2):
                                ch = half * 22 + f * 11 + jj
                                hid = half * 11 + jj
                                pz = PS()
                                for k in range(8):
                                    mm(pz[:, :], ws_up[:, k, half * 1408 + jj * 128:half * 1408 + (jj + 1) * 128], h2T[:, k, :], k == 0, k == 7,
                                       wsall + [h2T.res], [pz.res])
                                y = yb[half][pi % 2]
                                cv = cc_[half][pi % 2]
                                cvs.append(cv)
                                cw = o + 136 + ch * 3
                                cpy("pool", y[:, 0:2], halo[:, hid, :], [halo.res], [y.res])
                                cpy("act", y[:, 2:TB + 2], pz[:, :], [pz.res], [y.res])
                                act(cv[:, :], pz[:, :], AF.Identity, [pz.res, pp.res], [cv.res],
                                    scale=pp[:, cw + 2:cw + 3], bias=pp[:, o + 268 + ch:o + 269 + ch])
                                cpy("pool", halo[:, hid, :], y[:, TB:TB + 2], [y.res], [halo.res])
                                stt(cv[:, :], y[:, 1:TB + 1], pp[:, cw + 1:cw + 2], cv[:, :], ALU.mult, ALU.add, [y.res, pp.res, cv.res], [cv.res])
                                stt(cv[:, :], y[:, 0:TB], pp[:, cw:cw + 1], cv[:, :], ALU.mult, ALU.add, [y.res, pp.res, cv.res], [cv.res])
                            f_e = f_es[pi % 2]
                            f_s = f_ss[pi % 2]
                            pi += 1
                            act(f_e[:, :], cvs[0][:, :], AF.Exp, [cvs[0].res], [f_e.res], scale=-1.0)
                            act(f_e[:, :], f_e[:, :], AF.Ln, [f_e.res], [f_e.res], bias=1.0)
                            act(f_e[:, :], f_e[:, :], AF.Exp, [f_e.res], [f_e.res], scale=-1.0)
                            tt("dve", f_s[:, :], cvs[0][:, :], f_e[:, :], ALU.mult, [cvs[0].res, f_e.res], [f_s.res])
                            tt("dve", gT[:, jj, :], f_s[:, :], cvs[1][:, :], ALU.mult, [f_s.res, cvs[1].res], [gT.res])
                            if jj == 2:
                                while pend:
                                    pend.pop(0)()
                            if tb + 1 < NTB and jj in (3, 6, 8):
                                stage_in(tb + 1, {3: 0, 6: 1, 8: 2}[jj])

                        def down(tb=tb, tc=tc, xt=xt, gT=gT):
                            for m in range(8):
                                pd = PS()
                                for jj in range(11):
                                    mm(pd[:, :], ws_dn[:, jj, m * 128:(m + 1) * 128], gT[:, jj, :], jj == 0, jj == 10, wsall + [gT.res], [pd.res])
                                stt(xt[:, m, :], pd[:, :], g2[:, m:m + 1], xt[:, m, :], ALU.mult, ALU.add, [pd.res, modt.res, xt.res], [xt.res])
                            dma("sp", cp(xT_d)[:, :, tc], xt[:, :, :], reads=[xt.res], writes=[xres[tb]])
                        pend.append(down)
                    while pend:
                        pend.pop(0)()
                    P.end_phase()

        if stop_after is None and final:
            with ExitStack() as ph:
                xt = sbt(ph, "xt", [128, 8, TB], F32)
                ot = sbt(ph, "ot", [128, 8, TB], F32)
                sq = sbt(ph, "sq", [128, TB], BF16)
                lf = [sbt(ph, "lf%d" % i, [128, TB], F32) for i in range(3)]
                xfin = xT_in if nlayers == 0 else xT_d
                for tb in range(NTB):
                    tc = tcols(tb)
                    dma("sp", xt[:, :, :], cp(xfin)[:, :, tc], reads=[xres[tb]], writes=[xt.res])
                    rmsnorm_fm(xt, pp[:, L * PPL:L * PPL + 8], None, ot, True, sq, lf[0], lf[1], lf[2])
                    dma("sp", cp(outT)[:, :, tc], ot[:, :, :], reads=[ot.res], writes=[Res()])
                P.end_phase()
        print("total ops", P.total_ops, "max sem count/phase", P.maxcnt, P.dcnt)
    return nc


_CACHE = {}
NSPLIT = 1


def kernel(**inputs):
    inp = {k: np.ascontiguousarray(np.asarray(v, dtype=np.float32)) for k, v in inputs.items()}
    pp = _pack_pp(inp)
    bc = _pack_bc(inp)
    cf, cs = _consts()
    B = inp["x"].shape[0]
    per = L // NSPLIT
    xTs = [np.ascontiguousarray(inp["x"][b].T) for b in range(B)]
    for s in range(NSPLIT):
        last = (s == NSPLIT - 1)
        key = ("prog", s, NSPLIT)
        if key not in _CACHE:
            _CACHE[key] = build_program(nlayers=per, l0=s * per, final=last)
        nc = _CACHE[key]
        in_maps = []
        for b in range(B):
            in_maps.append({
                "xT": xTs[b],
                "cT": np.ascontiguousarray(inp["c"][b].reshape(8, 128).T),
                "ada_w": inp["ada_w"], "w_in": inp["w_in"], "w_out": inp["w_out"],
                "ffn_up": inp["ffn_up"], "ffn_down": inp["ffn_down"],
                "gla_w2": inp["gla_w2"], "sgu_w": inp["sgu_w"],
                "pp": pp, "bc": bc, "cf": cf, "cs": cs,
            })
        res = run_bass_kernel_spmd(nc, in_maps, core_ids=list(range(B)))
        if last:
            out = np.stack([np.ascontiguousarray(r["outT"].T) for r in res.results], axis=0)
        else:
            xTs = [np.ascontiguousarray(np.asarray(r["xT_d"], dtype=np.float32)) for r in res.results]
    return out.astype(np.float32)
```

```python
import os
import math
import numpy as np
from contextlib import ExitStack
import concourse.bass as bass
import concourse.mybir as mybir
from concourse.bass_utils import run_bass_kernel_spmd

F32 = mybir.dt.float32
BF16 = mybir.dt.bfloat16
ALU = mybir.AluOpType
AF = mybir.ActivationFunctionType
AX = mybir.AxisListType

L = 4
S = 4096
D = 1024
TB = 512
NTB = S // TB
NIN = 2576
NINX = NIN + 512
DFF = 2816
EPS = 1e-6
PPL = 312
NPP = L * PPL + 8
NBC = 896
NCF = 1156
GELU_K = 2.0 * math.sqrt(2.0 / math.pi)


class Res:
    __slots__ = ("w", "r")

    def __init__(self):
        self.w = None
        self.r = []


class Op:
    __slots__ = ("eng", "fn", "deps", "dma", "sig", "sigval", "sem", "semval", "semprev", "phase", "persist", "seq")

    def __init__(self, eng, fn, dma, phase, persist):
        self.eng = eng
        self.fn = fn
        self.dma = dma
        self.deps = []
        self.sig = False
        self.sigval = 0
        self.sem = None
        self.semval = 0
        self.semprev = 0
        self.phase = phase
        self.persist = persist
        self.seq = 0


class Tile:
    __slots__ = ("t", "res")

    def __init__(self, t):
        self.t = t
        self.res = Res()

    def __getitem__(self, k):
        return self.t[k]


class Prog:
    ENGS = ("pe", "act", "dve", "pool", "sp")
    CENGS = ("pe", "act", "dve", "pool")

    def __init__(self, nc, stack, n_dma=(("sp", 24), ("pool", 12))):
        self.nc = nc
        self.phase = 0
        self.ops = []
        self.esets = [{e: stack.enter_context(nc.semaphore("s%d_%s" % (i, e))) for e in self.CENGS} for i in range(3)]
        self.seqn = 0
        self.dsem = {q: [stack.enter_context(nc.semaphore("d_%s%d" % (q, i))) for i in range(n)] for q, n in n_dma}
        self.cnt = {e: 0 for e in self.CENGS}
        self.dcnt = {q: 0 for q in self.dsem}
        self.dval = {q: [0] * len(self.dsem[q]) for q in self.dsem}
        self.pending_persist = []
        self.total_ops = 0

    def op(self, eng, fn, reads=(), writes=(), dma=False, persist=False):
        o = Op(eng, fn, dma, self.phase, persist)
        self.seqn += 1
        o.seq = self.seqn
        deps = {}
        for r in reads:
            if r.w is not None:
                deps[id(r.w)] = r.w
        for w in writes:
            if w.w is not None:
                deps[id(w.w)] = w.w
            for q in w.r:
                deps[id(q)] = q
        for r in reads:
            if not dma:
                r.r = [q for q in r.r if q.dma or q.eng != eng]
            r.r.append(o)
        for w in writes:
            w.w = o
            w.r = []
        dl = []
        best = {}
        for d in deps.values():
            if d.phase != self.phase:
                if d.dma and d.persist:
                    dl.append(d)
                continue
            if d.dma:
                dl.append(d)
                continue
            if d.eng == "pe" and eng == "pe" and not dma:
                continue
            b = best.get(d.eng)
            if b is None or d.seq > b.seq:
                best[d.eng] = d
        dl.extend(best.values())
        o.deps = dl
        self.ops.append(o)
        return o

    def end_phase(self):
        nc = self.nc
        ops = self.ops
        self.total_ops += len(ops)
        self.esem = self.esets[self.phase % 3]
        nxt = self.esets[(self.phase + 1) % 3]
        self.cnt = {e: 0 for e in self.CENGS}
        for o in ops:
            for d in o.deps:
                if not d.dma:
                    d.sig = True
        streams = {e: [] for e in self.ENGS}
        for o in ops:
            streams[o.eng].append(o)
        last = {}
        for e in self.CENGS:
            for o in reversed(streams[e]):
                if not o.dma:
                    o.sig = True
                    last[e] = o
                    break
        for o in ops:
            if o.dma:
                q = o.eng
                i = self.dcnt[q] % len(self.dsem[q])
                self.dcnt[q] += 1
                o.sem = self.dsem[q][i]
                o.semprev = self.dval[q][i]
                self.dval[q][i] += 16
                o.semval = self.dval[q][i]
            elif o.sig:
                self.cnt[o.eng] += 1
                o.sigval = self.cnt[o.eng]
        bar = {}
        for o in ops:
            if o.dma and not o.persist:
                bar[id(o.sem)] = (o.sem, o.semval)
        for e, o in last.items():
            bar[id(self.esem[e])] = (self.esem[e], o.sigval)
        esem = self.esem

        def run(ename, eng):
            waited = {}
            if ename in nxt:
                eng.sem_clear(nxt[ename])
            for o in streams[ename]:
                if o.dma and o.semprev > 0:
                    k = id(o.sem)
                    if waited.get(k, 0) < o.semprev:
                        eng.wait_ge(o.sem, o.semprev)
                        waited[k] = o.semprev
                for d in o.deps:
                    if d.dma:
                        s, v = d.sem, d.semval
                    else:
                        s, v = esem[d.eng], d.sigval
                    k = id(s)
                    if waited.get(k, 0) < v:
                        eng.wait_ge(s, v)
                        waited[k] = v
                ins = o.fn(eng)
                if o.dma:
                    ins.then_inc(o.sem, 16)
                elif o.sig:
                    ins.then_inc(esem[ename], 1)
            for s, v in bar.values():
                if waited.get(id(s), 0) < v:
                    eng.wait_ge(s, v)

        with nc.Block() as block:
            @block.tensor
            def _(eng):
                run("pe", eng)

            @block.scalar
            def _(eng):
                run("act", eng)

            @block.vector
            def _(eng):
                run("dve", eng)

            @block.gpsimd
            def _(eng):
                run("pool", eng)

            @block.sync
            def _(eng):
                run("sp", eng)
        self.maxcnt = max(getattr(self, 'maxcnt', 0), max(self.cnt.values()))
        assert max(self.cnt.values()) < 3000, self.cnt
        self.ops = []
        self.phase += 1

    def final_wait_persist(self):
        return [(o.sem, o.semval) for o in self.pending_persist]


def _pack_pp(inp):
    pp = np.zeros((128, NPP), np.float32)
    for l in range(L):
        o = l * PPL
        pp[:, o + 0:o + 48] = inp["ada_b"][l].reshape(48, 128).T
        pp[:, o + 48:o + 56] = inp["ln1_g"][l].reshape(8, 128).T
        pp[:, o + 56:o + 64] = inp["ln2_g"][l].reshape(8, 128).T
        pp[:, o + 64:o + 126] = inp["conv_w"][l].reshape(31, 2, 128).transpose(2, 1, 0).reshape(128, 62)
        pp[:, o + 126:o + 128] = inp["conv_b"][l].reshape(2, 128).T
        pp[:, o + 128:o + 130] = inp["cln_g"][l].reshape(2, 128).T
        pp[:, o + 130:o + 132] = inp["cln_b"][l].reshape(2, 128).T
        pp[:, o + 132:o + 136] = inp["sgu_b"][l].T
        pp[:, o + 136:o + 268] = inp["ffn_conv_w"][l].reshape(3, 44, 128).transpose(2, 1, 0).reshape(128, 132)
        pp[:, o + 268:o + 312] = inp["ffn_conv_b"][l].reshape(44, 128).T
    pp[:, L * PPL:L * PPL + 8] = inp["lnf_g"].reshape(8, 128).T
    return pp


def _pack_bc(inp):
    bc = np.zeros((L, 128, NBC), np.float32)
    for l in range(L):
        row = np.concatenate([inp["gla_b"][l], np.tile(inp["gla_norm_g"][l], 4), inp["sgu_ln_g"][l], inp["sgu_ln_b"][l]])
        bc[l] = np.broadcast_to(row[None, :], (128, NBC))
    return bc


def _consts():
    cf = np.zeros((128, NCF), np.float32)
    i = np.arange(128)
    cf[:, 0:128] = np.eye(128)
    cf[:, 128:256] = (i[:, None] <= i[None, :]) / 16.0
    cf[:, 256:384] = (i[:, None] > i[None, :]) / 16.0
    cf[:, 384:512] = (i[:, None] <= i[None, :])
    cf[:, 512:640] = (i[:, None] >= i[None, :])
    cf[:, 640:896] = ((i[:, None] // 32) == (np.arange(256)[None, :] // 64))
    cf[:, 896:1024] = ((i[None, :] < 64) & (i[:, None] == i[None, :] + 64))
    cf[:, 1024:1152] = ((i[None, :] >= 64) & (i[:, None] == i[None, :] - 64))
    cf[:, 1152:1156] = ((i[:, None] // 32) == np.arange(4)[None, :])
    inv = (1.0 / (np.float32(10000.0) ** (np.arange(0, 64, 2, dtype=np.float32) / np.float32(64)))).astype(np.float32)
    ang = np.arange(S, dtype=np.float32)[:, None] * inv[None, :]
    cs = np.zeros((2, 128, S), np.float32)
    f = i % 32
    cs[0] = np.cos(ang).astype(np.float32).T[f]
    cs[1] = np.sin(ang).astype(np.float32).T[f]
    return cf, cs


def build_program(nlayers=L, debug=False, stop_after=None, p3test=None, l0=0, final=True):
    nc = bass.Bass("TRN2", target_bir_lowering=False)
    EI = "ExternalInput"
    xT_in = nc.dram_tensor("xT", [D, S], F32, kind=EI).ap()
    cT_in = nc.dram_tensor("cT", [128, 8], F32, kind=EI).ap()
    ada_w = nc.dram_tensor("ada_w", [L, D, 6 * D], F32, kind=EI).ap()
    w_in = nc.dram_tensor("w_in", [L, D, NIN], F32, kind=EI).ap()
    w_out = nc.dram_tensor("w_out", [L, D, D], F32, kind=EI).ap()
    ffn_up = nc.dram_tensor("ffn_up", [L, D, 2 * DFF], F32, kind=EI).ap()
    ffn_down = nc.dram_tensor("ffn_down", [L, DFF, D], F32, kind=EI).ap()
    gla_w2 = nc.dram_tensor("gla_w2", [L, 16, 128], F32, kind=EI).ap()
    sgu_w = nc.dram_tensor("sgu_w", [L, 4, 128, 128], F32, kind=EI).ap()
    pp_in = nc.dram_tensor("pp", [128, NPP], F32, kind=EI).ap()
    bc_in = nc.dram_tensor("bc", [L, 128, NBC], F32, kind=EI).ap()
    cf_in = nc.dram_tensor("cf", [128, NCF], F32, kind=EI).ap()
    cs_in = nc.dram_tensor("cs", [2, 128, S], F32, kind=EI).ap()
    outT = nc.dram_tensor("outT", [D, S], F32, kind="ExternalOutput").ap() if final else None
    SK = "ExternalOutput" if debug else "Internal"
    xT_d = nc.dram_tensor("xT_d", [D, S], F32, kind=(SK if final else "ExternalOutput")).ap()
    oT_d = nc.dram_tensor("oT_d", [D, S], BF16, kind=SK).ap()
    qkv_d = nc.dram_tensor("qkv_d", [3, 256, S], BF16, kind=(EI if p3test else SK)).ap()
    h2T_d = nc.dram_tensor("h2T_d", [D, S], BF16, kind=SK).ap()

    def cp(ap):
        return ap.rearrange("(c p) t -> p c t", p=128)

    with ExitStack() as st:
        P = Prog(nc, st)

        uid = [0]

        def sbt(stack, name, shape, dt):
            uid[0] += 1
            return Tile(stack.enter_context(nc.sbuf_tensor("sb%d_%s" % (uid[0], name), shape, dt)))

        def pst(stack, name, shape, dt):
            return Tile(stack.enter_context(nc.psum_tensor("ps_" + name, shape, dt)))

        cf = sbt(st, "cf", [128, NCF], F32)
        pp = sbt(st, "pp", [128, NPP], F32)
        modt = sbt(st, "mod", [128, L, 48], F32)
        gm = sbt(st, "gm", [128, L, 16], F32)
        identb = sbt(st, "identb", [128, 128], BF16)
        onesb = sbt(st, "onesb", [128, 128], BF16)
        onesf = sbt(st, "onesf", [128, 128], F32)
        maskCb = sbt(st, "maskCb", [128, 256], BF16)
        maskLE4 = sbt(st, "maskLE4", [128, 512], F32)
        ws = sbt(st, "ws", [128, 34560], BF16)
        psf = [pst(st, "psf%d" % i, [128, 512], F32) for i in range(6)]
        psb = [pst(st, "psb%d" % i, [128, 1024], BF16) for i in range(2)]
        psi = [0, 0]

        def PS():
            t = psf[psi[0] % len(psf)]
            psi[0] += 1
            return t

        def PSB():
            t = psb[psi[1] % len(psb)]
            psi[1] += 1
            return t

        ws_in = ws[:, 0:8 * NINX].rearrange("p (k n) -> p k n", k=8)
        ws_out = ws[:, 8 * NINX:8 * NINX + 8192].rearrange("p (k n) -> p k n", k=8)
        ws_up = ws[:, 0:22528].rearrange("p (k n) -> p k n", k=8)
        ws_dn = ws[:, 22528:33792].rearrange("p (j n) -> p j n", j=11)

        ident = cf[:, 0:128]
        triT = cf[:, 128:256]
        triS = cf[:, 256:384]
        maskLE = cf[:, 384:512]
        blockmask = cf[:, 640:896]
        swapLo = cf[:, 896:1024]
        swapHi = cf[:, 1024:1152]
        headmask = cf[:, 1152:1156]

        wsr = [Res() for _ in range(40)]
        wsrot = Res()
        wsall = wsr + [wsrot]
        xres = [Res() for _ in range(NTB)]
        ores = {}
        qkvres = {}
        h2res = [Res() for _ in range(NTB)]

        def R(dct, key):
            if key not in dct:
                dct[key] = Res()
            return dct[key]

        def tcols(tb):
            return slice(tb * TB, (tb + 1) * TB)

        def act(out, in_, func, reads, writes, scale=1.0, bias=0.0):
            P.op("act", lambda e: e.activation(out=out, in_=in_, func=func, scale=scale, bias=bias), reads=reads, writes=writes)

        def dma(q, out, in_, reads=(), writes=(), persist=False):
            P.op(q, lambda e: e.dma_start(out=out, in_=in_), reads=reads, writes=writes, dma=True, persist=persist)

        def mm(out, lhsT, rhs, start, stop, reads, writes):
            P.op("pe", lambda e: e.matmul(out, lhsT=lhsT, rhs=rhs, start=start, stop=stop), reads=reads, writes=writes)

        def tt(eng, out, in0, in1, op, reads, writes):
            P.op(eng, lambda e: e.tensor_tensor(out=out, in0=in0, in1=in1, op=op), reads=reads, writes=writes)

        def ts(eng, out, in0, s1, s2, op0, op1, reads, writes):
            P.op(eng, lambda e: e.tensor_scalar(out=out, in0=in0, scalar1=s1, scalar2=s2, op0=op0, op1=op1), reads=reads, writes=writes)

        def stt(out, in0, scalar, in1, op0, op1, reads, writes):
            P.op("dve", lambda e: e.scalar_tensor_tensor(out=out, in0=in0, scalar=scalar, in1=in1, op0=op0, op1=op1), reads=reads, writes=writes)

        def recip(out, in_, reads, writes):
            P.op("dve", lambda e: e.reciprocal(out=out, in_=in_), reads=reads, writes=writes)

        def cpy(eng, out, in_, reads, writes):
            if eng == "act":
                act(out, in_, AF.Copy, reads, writes)
            else:
                P.op(eng, lambda e: e.tensor_copy(out=out, in_=in_), reads=reads, writes=writes)

        def memset(eng, ap, val, writes):
            P.op(eng, lambda e: e.memset(ap, val), writes=writes)

        def rmsnorm_fm(xt, gmcol, shcol, outT_tile, out_dt_f32, sq, f1, f2, f3):
            pssum = PS()
            for c in range(8):
                act(sq[:, :], xt[:, c, :], AF.Square, [xt.res], [sq.res])
                mm(pssum[:, :], onesb[:, :], sq[:, :], c == 0, c == 7, [onesb.res, sq.res], [pssum.res])
            act(f1[:, :], pssum[:, :], AF.Ln, [pssum.res], [f1.res], scale=1.0 / D, bias=EPS)
            act(f2[:, :], f1[:, :], AF.Exp, [f1.res], [f2.res], scale=-0.5)
            for c in range(8):
                if shcol is None:
                    stt(outT_tile[:, c, :], xt[:, c, :], gmcol[:, c:c + 1], f2[:, :], ALU.mult, ALU.mult,
                        [xt.res, f2.res, pp.res, gm.res], [outT_tile.res])
                else:
                    stt(f3[:, :], xt[:, c, :], gmcol[:, c:c + 1], f2[:, :], ALU.mult, ALU.mult,
                        [xt.res, f2.res, pp.res, gm.res], [f3.res])
                    ts("pool", outT_tile[:, c, :], f3[:, :], shcol[:, c:c + 1], 1.0, ALU.add, ALU.mult,
                       [f3.res, modt.res], [outT_tile.res])

        with ExitStack() as ph:
            c_sb = sbt(ph, "c_sb", [128, 8], F32)
            t8a = sbt(ph, "t8a", [128, 8], F32)
            t8b = sbt(ph, "t8b", [128, 8], F32)
            cond = sbt(ph, "cond", [128, 8], F32)
            aw = [sbt(ph, "aw%d" % i, [128, 8, 256], F32) for i in range(2)]
            modrow = sbt(ph, "modrow", [1, 6 * D], F32)
            dma("sp", cf[:, :], cf_in, writes=[cf.res])
            dma("sp", pp[:, :], pp_in, writes=[pp.res])
            dma("sp", c_sb[:, :], cT_in, writes=[c_sb.res])
            cpy("dve", identb[:, :], ident, [cf.res], [identb.res])
            memset("dve", onesb[:, :], 1.0, [onesb.res])
            memset("pool", onesf[:, :], 1.0, [onesf.res])
            cpy("dve", maskCb[:, 0:128], cf[:, 512:640], [cf.res], [maskCb.res])
            cpy("dve", maskCb[:, 128:256], cf[:, 384:512], [cf.res], [maskCb.res])
            for h in range(4):
                cpy("pool", maskLE4[:, h * 128:(h + 1) * 128], maskLE, [cf.res], [maskLE4.res])
            act(t8a[:, :], c_sb[:, :], AF.Exp, [c_sb.res], [t8a.res], scale=-1.0)
            ts("dve", t8a[:, :], t8a[:, :], 1.0, None, ALU.add, ALU.bypass, [t8a.res], [t8a.res])
            recip(t8b[:, :], t8a[:, :], [t8a.res], [t8b.res])
            tt("dve", cond[:, :], c_sb[:, :], t8b[:, :], ALU.mult, [c_sb.res, t8b.res], [cond.res])
            for l in (range(0) if p3test else range(l0, l0 + nlayers)):
                for n in range(24):
                    a = aw[n % 2]
                    dma("sp", a[:, :, :], ada_w[l][:, n * 256:(n + 1) * 256].rearrange("(k p) n -> p k n", p=128), writes=[a.res])
                    pr = PS()
                    for k in range(8):
                        mm(pr[0:1, 0:256], cond[:, k:k + 1], a[:, k, :], k == 0, k == 7, [cond.res, a.res], [pr.res])
                    cpy("act", modrow[0:1, n * 256:(n + 1) * 256], pr[0:1, 0:256], [pr.res], [modrow.res])
                pm = PS()
                for m in range(48):
                    mm(pm[:, m:m + 1], modrow[0:1, m * 128:(m + 1) * 128], cf[0:1, 0:1], True, True, [modrow.res, cf.res], [pm.res])
                o = l * PPL
                tt("dve", modt[:, l, :], pm[:, 0:48], pp[:, o:o + 48], ALU.add, [pm.res, pp.res], [modt.res])
                stt(gm[:, l, 0:8], modt[:, l, 8:16], 1.0, pp[:, o + 48:o + 56], ALU.add, ALU.mult, [modt.res, pp.res], [gm.res])
                stt(gm[:, l, 8:16], modt[:, l, 32:40], 1.0, pp[:, o + 56:o + 64], ALU.add, ALU.mult, [modt.res, pp.res], [gm.res])
            P.end_phase()

        for l in range(l0, l0 + nlayers):
            o = l * PPL
            xsrc = xT_in if l == l0 else xT_d
            sh1 = modt[:, l, 0:8]
            g1 = modt[:, l, 16:24]
            sh2 = modt[:, l, 24:32]
            g2 = modt[:, l, 40:48]
            with ExitStack() as ph:
              if not p3test:
                diag = sbt(ph, "diag", [128, 2, 31, 128], BF16)
                xt = sbt(ph, "xt", [128, 8, TB], F32)
                hTs = [sbt(ph, "hT%d" % i, [128, 8, TB], BF16) for i in range(2)]
                hA = [sbt(ph, "hA%d" % i, [128, 2, TB + 30], BF16) for i in range(2)]
                bcs = sbt(ph, "bcs", [128, NBC], F32)
                w2s = sbt(ph, "w2s", [16, 128], F32)
                sgWT = sbt(ph, "sgWT", [128, 4, 128], BF16)
                Sfull = sbt(ph, "Sfull", [128, 256], F32)
                Sm = [sbt(ph, "Sm%d" % i, [128, 256], BF16) for i in range(2)]
                sq = sbt(ph, "sq", [128, TB], BF16)
                lf = [sbt(ph, "lf%d" % i, [128, TB], F32) for i in range(3)]
                a_e = sbt(ph, "a_e", [128, TB], F32)
                yA = [sbt(ph, "yA%d" % i, [128, TB], F32) for i in range(2)]
                a_yb = sbt(ph, "a_yb", [128, TB], BF16)
                a_sb = sbt(ph, "a_sb", [128, TB], BF16)
                a_m = sbt(ph, "a_m", [128, TB], F32)
                a_m2 = sbt(ph, "a_m2", [128, TB], F32)
                a_rs = sbt(ph, "a_rs", [128, TB], F32)
                a_d = sbt(ph, "a_d", [128, TB], F32)
                a_o = sbt(ph, "a_o", [128, TB], BF16)
                lrs = sbt(ph, "lrs", [16, TB], F32)
                qbs = sbt(ph, "qbs", [128, TB], F32)
                kbs = sbt(ph, "kbs", [128, TB], F32)
                b_e1 = sbt(ph, "b_e1", [128, 128], F32)
                b_sp = sbt(ph, "b_sp", [128, 128], F32)
                b_E2 = [sbt(ph, "b_E%d" % i, [128, 384], F32) for i in range(2)]
                b_qt2 = [sbt(ph, "b_qt%d" % i, [128, 128], BF16) for i in range(2)]
                b_kt = sbt(ph, "b_kt", [128, 128], BF16)
                b_k42 = [sbt(ph, "b_k4%d" % i, [128, 512], BF16) for i in range(2)]
                b_ke2 = [sbt(ph, "b_ke%d" % i, [128, 128], BF16) for i in range(2)]
                b_kf = sbt(ph, "b_kf", [128, 128], F32)
                b_v2 = [sbt(ph, "b_v%d" % i, [128, 256], BF16) for i in range(2)]
                b_at = sbt(ph, "b_at", [128, 512], BF16)
                b_osq = sbt(ph, "b_osq", [128, 256], F32)
                b_s4 = sbt(ph, "b_s4", [128, 12], F32)
                b_eg = sbt(ph, "b_eg", [128, 256], F32)
                b_gt = sbt(ph, "b_gt", [128, 256], F32)
                b_ob = sbt(ph, "b_ob", [128, 256], BF16)
                oBT = sbt(ph, "oBT", [128, 2, TB], BF16)
                d_xs = sbt(ph, "d_xs", [128, 512], F32)
                d_a = sbt(ph, "d_a", [128, 512], F32)
                d_b = sbt(ph, "d_b", [128, 512], F32)
                d_ge = sbt(ph, "d_ge", [128, 512], F32)
                d_st = sbt(ph, "d_st", [128, 16], F32)
                d_vn = sbt(ph, "d_vn", [128, 256], F32)
                d_vl = sbt(ph, "d_vl", [128, 256], BF16)
                d_od = sbt(ph, "d_od", [128, 256], BF16)
                oDT = sbt(ph, "oDT", [128, 2, TB], BF16)
                csc = sbt(ph, "csc", [128, TB], F32)
                css = sbt(ph, "css", [128, TB], F32)
                c_t1 = sbt(ph, "c_t1", [128, TB], F32)
                c_t2 = sbt(ph, "c_t2", [128, TB], F32)
                c_o = [sbt(ph, "c_o%d" % i, [128, TB], BF16) for i in range(2)]

                for k in range(8):
                    dma("pool", ws_in[:, k, 0:NIN], w_in[l][k * 128:(k + 1) * 128, :], writes=[wsr[k]])
                dma("sp", bcs[:, :], bc_in[l], writes=[bcs.res])
                dma("sp", w2s[:, :], gla_w2[l], writes=[w2s.res])
                dma("sp", d_xs[:, :].rearrange("p (g s) -> p g s", g=4), sgu_w[l].rearrange("g t s -> t g s"), writes=[d_xs.res])
                for qi, c0 in enumerate((1296, 1552)):
                    r0 = NIN + qi * 256
                    for k in range(8):
                        src = ws_in[:, k, c0:c0 + 256].rearrange("p (h t r) -> p h t r", h=4, t=2)
                        dst = ws_in[:, k, r0:r0 + 256].rearrange("p (h t r) -> p h t r", h=4, t=2)
                        ts("dve" if k % 2 == 0 else "pool", dst[:, :, 0, :], src[:, :, 1, :], -1.0, 1.0, ALU.mult, ALU.mult, [wsr[k]], [wsrot])
                        cpy("pool" if k % 2 == 0 else "dve", dst[:, :, 1, :], src[:, :, 0, :], [wsr[k]], [wsrot])
                for c in range(2):
                    for k in range(31):
                        ts("dve" if (k % 2 == 0) else "pool", diag[:, c, k, :], ident, pp[:, o + 64 + c * 31 + k:o + 64 + c * 31 + k + 1], 1.0,
                           ALU.mult, ALU.mult, [cf.res, pp.res], [diag.res])
                psg = PS()
                for g in range(4):
                    P.op("pe", lambda e, g=g: e.transpose(out=psg[:, g * 128:(g + 1) * 128], in_=d_xs[:, g * 128:(g + 1) * 128], identity=ident),
                         reads=[d_xs.res, cf.res], writes=[psg.res])
                tt("dve", sgWT[:, :, :].rearrange("p g t -> p (g t)"), psg[:, :], maskLE4[:, :], ALU.mult, [psg.res, maskLE4.res], [sgWT.res])
                memset("dve", Sfull[:, :], 0.0, [Sfull.res])
                memset("pool", Sm[0][:, :], 0.0, [Sm[0].res])
                memset("pool", hA[0][:, :, 0:30], 0.0, [hA[0].res])
                memset("pool", hA[1][:, :, 0:30], 0.0, [hA[1].res])

                def fm(hT, c0, M=128):
                    pt = PS()
                    for k in range(8):
                        mm(pt[0:M, :], ws_in[:, k, c0:c0 + M], hT[:, k, :], k == 0, k == 7, wsall + [hT.res], [pt.res])
                    return pt

                def tm(hT, tt_, c0, N):
                    pt = PS()
                    for k in range(8):
                        mm(pt[:, 0:N], hT[:, k, tt_ * 128:(tt_ + 1) * 128], ws_in[:, k, c0:c0 + N], k == 0, k == 7, wsall + [hT.res], [pt.res])
                    return pt

                schunk = [0]

                def sigm(t, src, reads):
                    act(t[:, :], src, AF.Exp, reads, [t.res], scale=-1.0)
                    act(t[:, :], t[:, :], AF.Ln, [t.res], [t.res], bias=1.0)
                    act(t[:, :], t[:, :], AF.Exp, [t.res], [t.res], scale=-1.0)

                def genA(tb, tc):
                    hT = hTs[tb % 2]
                    hcur = hA[tb % 2]
                    hprev = hA[(tb + 1) % 2]
                    if tb > 0:
                        cpy("pool", hcur[:, :, 0:30], hprev[:, :, TB:TB + 30], [hprev.res], [hcur.res])
                    for c in range(2):
                        pv = fm(hT, c * 128)
                        pg = fm(hT, 256 + c * 128)
                        sigm(a_e, pg[:, :], [pg.res])
                        tt("dve", hcur[:, c, 30:30 + TB], pv[:, :], a_e[:, :], ALU.mult, [pv.res, a_e.res], [hcur.res])
                        yield
                        py = PS()
                        for k in range(31):
                            mm(py[:, :], diag[:, c, k, :], hcur[:, c, k:k + TB], k == 0, k == 30, [diag.res, hcur.res], [py.res])
                        act(yA[c][:, :], py[:, :], AF.Identity, [py.res, pp.res], [yA[c].res], bias=pp[:, o + 126 + c:o + 127 + c])
                        yield
                    pmean = PS()
                    pmsq = PS()
                    for c in range(2):
                        cpy("dve", a_yb[:, :], yA[c][:, :], [yA[c].res], [a_yb.res])
                        act(a_sb[:, :], yA[c][:, :], AF.Square, [yA[c].res], [a_sb.res])
                        mm(pmean[:, :], onesb[:, :], a_yb[:, :], c == 0, c == 1, [onesb.res, a_yb.res], [pmean.res])
                        mm(pmsq[:, :], onesb[:, :], a_sb[:, :], c == 0, c == 1, [onesb.res, a_sb.res], [pmsq.res])
                    act(a_m[:, :], pmean[:, :], AF.Copy, [pmean.res], [a_m.res], scale=1.0 / 256)
                    tt("dve", a_m2[:, :], a_m[:, :], a_m[:, :], ALU.mult, [a_m.res], [a_m2.res])
                    stt(a_m2[:, :], pmsq[:, :], 1.0 / 256, a_m2[:, :], ALU.mult, ALU.subtract, [pmsq.res, a_m2.res], [a_m2.res])
                    act(a_rs[:, :], a_m2[:, :], AF.Ln, [a_m2.res], [a_rs.res], bias=EPS)
                    act(a_rs[:, :], a_rs[:, :], AF.Exp, [a_rs.res], [a_rs.res], scale=-0.5)
                    yield
                    for c in range(2):
                        tt("dve", a_d[:, :], yA[c][:, :], a_m[:, :], ALU.subtract, [yA[c].res, a_m.res], [a_d.res])
                        tt("dve", a_d[:, :], a_d[:, :], a_rs[:, :], ALU.mult, [a_d.res, a_rs.res], [a_d.res])
                        act(a_d[:, :], a_d[:, :], AF.Identity, [a_d.res, pp.res], [a_d.res],
                            scale=pp[:, o + 128 + c:o + 129 + c], bias=pp[:, o + 130 + c:o + 131 + c])
                        sigm(a_e, a_d[:, :], [a_d.res])
                        tt("dve", a_o[:, :], a_d[:, :], a_e[:, :], ALU.mult, [a_d.res, a_e.res], [a_o.res])
                        dma("sp", oT_d[c * 128:(c + 1) * 128, tc], a_o[:, :], reads=[a_o.res], writes=[R(ores, (c, tb))])
                        yield

                bfront_done = {}
                bback_done = {}

                def genB(tb, tc):
                    hT = hTs[tb % 2]
                    pq = fm(hT, 512)
                    cpy("act", qbs[:, :], pq[:, :], [pq.res], [qbs.res])
                    pk = fm(hT, 640)
                    cpy("act", kbs[:, :], pk[:, :], [pk.res], [kbs.res])
                    plr = fm(hT, 1280, 16)
                    cpy("act", lrs[:, :], plr[0:16, :], [plr.res], [lrs.res])
                    yield
                    for t4 in range(4):
                        t0 = t4 * 128
                        tsl = slice(t0, t0 + 128)
                        i2 = t4 % 2
                        gidx = tb * 4 + t4
                        while gidx >= 2 and (gidx - 2) not in bback_done:
                            yield
                        pkv = tm(hT, t4, 640, 384)
                        cpy("act", b_v2[i2][:, :], pkv[:, 128:384], [pkv.res], [b_v2[i2].res])
                        cpy("act", b_kf[:, :], pkv[:, 0:128], [pkv.res], [b_kf.res])
                        pgk = PS()
                        mm(pgk[:, 0:128], lrs[0:16, tsl], w2s[0:16, :], True, False, [lrs.res, w2s.res], [pgk.res])
                        mm(pgk[:, 0:128], onesf[0:1, 0:128], bcs[0:1, 0:128], False, True, [onesf.res, bcs.res], [pgk.res])
                        act(b_e1[:, :], pgk[:, 0:128], AF.Exp, [pgk.res], [b_e1.res], scale=-1.0)
                        act(b_sp[:, :], b_e1[:, :], AF.Ln, [b_e1.res], [b_sp.res], bias=1.0)
                        yield
                        bE = b_E2[i2]
                        pB = PS()
                        mm(pB[:, 0:128], b_sp[:, :], triT, True, True, [b_sp.res, cf.res], [pB.res])
                        mm(pB[:, 128:256], triS, b_sp[:, :], True, True, [b_sp.res, cf.res], [pB.res])
                        act(bE[:, 0:128], pB[:, 0:128], AF.Exp, [pB.res], [bE.res], scale=-1.0)
                        act(bE[:, 128:256], pB[:, 0:128], AF.Exp, [pB.res], [bE.res], scale=1.0)
                        act(bE[:, 256:384], pB[:, 128:256], AF.Exp, [pB.res], [bE.res], scale=-1.0)
                        yield
                        stt(b_qt2[i2][:, :], qbs[:, tsl], 32.0 ** -0.5, bE[:, 0:128], ALU.mult, ALU.mult, [qbs.res, bE.res], [b_qt2[i2].res])
                        tt("dve", b_kt[:, :], kbs[:, tsl], bE[:, 128:256], ALU.mult, [kbs.res, bE.res], [b_kt.res])
                        for h in range(4):
                            ts("pool", b_k42[i2][:, h * 128:(h + 1) * 128], b_kt[:, :], headmask[:, h:h + 1], 1.0, ALU.mult, ALU.mult,
                               [b_kt.res, cf.res], [b_k42[i2].res])
                        tt("dve", b_ke2[i2][:, :], b_kf[:, :], bE[:, 256:384], ALU.mult, [b_kf.res, bE.res], [b_ke2[i2].res])
                        bfront_done[(tb, t4)] = True
                        yield

                def genBb(tb, tc):
                    hT = hTs[tb % 2]
                    for t4 in range(4):
                        t0 = t4 * 128
                        tsl = slice(t0, t0 + 128)
                        i2 = t4 % 2
                        while (tb, t4) not in bfront_done:
                            yield
                        bE = b_E2[i2]
                        b_qt = b_qt2[i2]
                        b_k4 = b_k42[i2]
                        b_ke = b_ke2[i2]
                        b_v = b_v2[i2]
                        patt = PS()
                        for h in range(4):
                            mm(patt[:, h * 128:(h + 1) * 128], b_k4[:, h * 128:(h + 1) * 128], b_qt[:, :], True, True,
                               [b_k4.res, b_qt.res], [patt.res])
                        tt("dve", b_at[:, :], patt[:, :], maskLE4[:, :], ALU.mult, [patt.res, maskLE4.res], [b_at.res])
                        yield
                        smp = Sm[schunk[0] % 2]
                        smn = Sm[(schunk[0] + 1) % 2]
                        schunk[0] += 1
                        po = PS()
                        mm(po[:, 0:256], b_qt[:, :], smp[:, :], True, False, [b_qt.res, smp.res], [po.res])
                        for h in range(4):
                            mm(po[:, h * 64:(h + 1) * 64], b_at[:, h * 128:(h + 1) * 128], b_v[:, h * 64:(h + 1) * 64], False, h == 3,
                               [b_at.res, b_v.res], [po.res])
                        pdS = PS()
                        mm(pdS[:, 0:256], b_ke[:, :], b_v[:, :], True, True, [b_ke.res, b_v.res], [pdS.res])
                        stt(Sfull[:, :], Sfull[:, :], bE[:, 127:128], pdS[:, 0:256], ALU.mult, ALU.add, [Sfull.res, bE.res, pdS.res], [Sfull.res])
                        tt("pool", smn[:, :], Sfull[:, :], blockmask, ALU.mult, [Sfull.res, cf.res], [smn.res])
                        pgt = tm(hT, t4, 1024, 256)
                        act(b_osq[:, :], po[:, 0:256], AF.Square, [po.res], [b_osq.res])
                        P.op("dve", lambda e: e.reduce_sum(out=b_s4[:, 0:4], in_=b_osq[:, :].rearrange("p (h e) -> p h e", h=4), axis=AX.X),
                             reads=[b_osq.res], writes=[b_s4.res])
                        act(b_s4[:, 4:8], b_s4[:, 0:4], AF.Ln, [b_s4.res], [b_s4.res], scale=1.0 / 64, bias=EPS)
                        act(b_s4[:, 8:12], b_s4[:, 4:8], AF.Exp, [b_s4.res], [b_s4.res], scale=-0.5)
                        sigm(b_eg, pgt[:, 0:256], [pgt.res])
                        tt("dve", b_gt[:, :], pgt[:, 0:256], b_eg[:, :], ALU.mult, [pgt.res, b_eg.res], [b_gt.res])
                        tt("pool", b_gt[:, :], b_gt[:, :], bcs[:, 128:384], ALU.mult, [b_gt.res, bcs.res], [b_gt.res])
                        for h in range(4):
                            stt(b_ob[:, h * 64:(h + 1) * 64], po[:, h * 64:(h + 1) * 64], b_s4[:, 8 + h:9 + h], b_gt[:, h * 64:(h + 1) * 64],
                                ALU.mult, ALU.mult, [po.res, b_s4.res, b_gt.res], [b_ob.res])
                        yield
                        yield
                        yield
                        yield
                        ptr = PSB()
                        for cc in range(2):
                            P.op("pe", lambda e, cc=cc, ptr=ptr: e.transpose(out=ptr[:, cc * 128:(cc + 1) * 128], in_=b_ob[:, cc * 128:(cc + 1) * 128], identity=identb[:, :]),
                                 reads=[b_ob.res, identb.res], writes=[ptr.res])
                        cpy("act", oBT[:, :, tsl], ptr[:, 0:256].rearrange("p (c t) -> p c t", c=2), [ptr.res], [oBT.res])
                        bback_done[tb * 4 + t4] = True
                        yield
                    dma("sp", cp(oT_d[256:512, :])[:, :, tc], oBT[:, :, :], reads=[oBT.res], writes=[R(ores, (2, tb)), R(ores, (3, tb))])

                def genD(tb, tc):
                    hT = hTs[tb % 2]
                    for t4 in range(4):
                        t0 = t4 * 128
                        tsl = slice(t0, t0 + 128)
                        puv = tm(hT, t4, 2064, 512)
                        cpy("act", d_xs[:, :], puv[:, :], [puv.res], [d_xs.res])
                        yield
                        tt("pool", d_a[:, :], d_xs[:, :], d_xs[:, :], ALU.mult, [d_xs.res], [d_a.res])
                        ts("pool", d_a[:, :], d_a[:, :], 0.044715, 1.0, ALU.mult, ALU.add, [d_a.res], [d_a.res])
                        tt("dve", d_a[:, :], d_a[:, :], d_xs[:, :], ALU.mult, [d_a.res, d_xs.res], [d_a.res])
                        act(d_b[:, :], d_a[:, :], AF.Exp, [d_a.res], [d_b.res], scale=-GELU_K)
                        act(d_b[:, :], d_b[:, :], AF.Ln, [d_b.res], [d_b.res], bias=1.0)
                        act(d_b[:, :], d_b[:, :], AF.Exp, [d_b.res], [d_b.res], scale=-1.0)
                        tt("dve", d_ge[:, :], d_xs[:, :], d_b[:, :], ALU.mult, [d_xs.res, d_b.res], [d_ge.res])
                        yield
                        P.op("dve", lambda e: e.bn_stats(out=d_st[:, 0:6], in_=d_ge[:, 256:512]), reads=[d_ge.res], writes=[d_st.res])
                        P.op("dve", lambda e: e.bn_aggr(out=d_st[:, 8:10], in_=d_st[:, 0:6]), reads=[d_st.res], writes=[d_st.res])
                        act(d_st[:, 10:11], d_st[:, 9:10], AF.Ln, [d_st.res], [d_st.res], bias=EPS)
                        act(d_st[:, 11:12], d_st[:, 10:11], AF.Exp, [d_st.res], [d_st.res], scale=-0.5)
                        ts("dve", d_vn[:, :], d_ge[:, 256:512], d_st[:, 8:9], d_st[:, 11:12], ALU.subtract, ALU.mult, [d_ge.res, d_st.res], [d_vn.res])
                        tt("dve", d_vn[:, :], d_vn[:, :], bcs[:, 384:640], ALU.mult, [d_vn.res, bcs.res], [d_vn.res])
                        tt("pool", d_vl[:, :], d_vn[:, :], bcs[:, 640:896], ALU.add, [d_vn.res, bcs.res], [d_vl.res])
                        yield
                        yield
                        yield
                        psv = PS()
                        for g in range(4):
                            mm(psv[:, g * 64:(g + 1) * 64], sgWT[:, g, :], d_vl[:, g * 64:(g + 1) * 64], True, True, [sgWT.res, d_vl.res], [psv.res])
                        for g in range(4):
                            stt(d_od[:, g * 64:(g + 1) * 64], psv[:, g * 64:(g + 1) * 64], pp[:, o + 132 + g:o + 133 + g], d_ge[:, g * 64:(g + 1) * 64],
                                ALU.add, ALU.mult, [psv.res, pp.res, d_ge.res], [d_od.res])
                        yield
                        yield
                        yield
                        ptr = PSB()
                        for cc in range(2):
                            P.op("pe", lambda e, cc=cc, ptr=ptr: e.transpose(out=ptr[:, cc * 128:(cc + 1) * 128], in_=d_od[:, cc * 128:(cc + 1) * 128], identity=identb[:, :]),
                                 reads=[d_od.res, identb.res], writes=[ptr.res])
                        cpy("act", oDT[:, :, tsl], ptr[:, 0:256].rearrange("p (c t) -> p c t", c=2), [ptr.res], [oDT.res])
                        yield
                    dma("sp", cp(oT_d[768:1024, :])[:, :, tc], oDT[:, :, :], reads=[oDT.res], writes=[R(ores, (6, tb)), R(ores, (7, tb))])

                def genC(tb, tc):
                    hT = hTs[tb % 2]
                    dma("sp", csc[:, :], cs_in[0][:, tc], writes=[csc.res])
                    dma("sp", css[:, :], cs_in[1][:, tc], writes=[css.res])
                    ci = 0
                    for qi, c0 in enumerate((1296, 1552)):
                        r0 = NIN + qi * 256
                        for c in range(2):
                            p1 = fm(hT, c0 + c * 128)
                            tt("dve", c_t1[:, :], p1[:, :], csc[:, :], ALU.mult, [p1.res, csc.res], [c_t1.res])
                            p2 = fm(hT, r0 + c * 128)
                            tt("dve", c_t2[:, :], p2[:, :], css[:, :], ALU.mult, [p2.res, css.res], [c_t2.res])
                            co = c_o[ci % 2]
                            ci += 1
                            tt("pool", co[:, :], c_t1[:, :], c_t2[:, :], ALU.add, [c_t1.res, c_t2.res], [co.res])
                            dma("sp", qkv_d[qi][c * 128:(c + 1) * 128, tc], co[:, :], reads=[co.res], writes=[R(qkvres, (qi, c))])
                            yield
                    for c in range(2):
                        p1 = fm(hT, 1808 + c * 128)
                        co = c_o[ci % 2]
                        ci += 1
                        cpy("act", co[:, :], p1[:, :], [p1.res], [co.res])
                        dma("sp", qkv_d[2][c * 128:(c + 1) * 128, tc], co[:, :], reads=[co.res], writes=[R(qkvres, (2, c))])
                        yield

                def genLN(tb, tc):
                    hT = hTs[tb % 2]
                    dma("sp", xt[:, :, :], cp(xsrc)[:, :, tc], reads=[xres[tb]], writes=[xt.res])
                    yield
                    pssum = PS()
                    for c in range(8):
                        act(sq[:, :], xt[:, c, :], AF.Square, [xt.res], [sq.res])
                        mm(pssum[:, :], onesb[:, :], sq[:, :], c == 0, c == 7, [onesb.res, sq.res], [pssum.res])
                    act(lf[0][:, :], pssum[:, :], AF.Ln, [pssum.res], [lf[0].res], scale=1.0 / D, bias=EPS)
                    act(lf[1][:, :], lf[0][:, :], AF.Exp, [lf[0].res], [lf[1].res], scale=-0.5)
                    yield
                    for c in range(8):
                        f3 = lf[0] if c % 2 == 0 else lf[2]
                        stt(f3[:, :], xt[:, c, :], gm[:, l, c:c + 1], lf[1][:, :], ALU.mult, ALU.mult, [xt.res, lf[1].res, gm.res], [f3.res])
                        ts("pool", hT[:, c, :], f3[:, :], sh1[:, c:c + 1], 1.0, ALU.add, ALU.mult, [f3.res, modt.res], [hT.res])
                        if c % 2 == 1:
                            yield

                makers = {"A": genA, "B": genB, "Bb": genBb, "D": genD, "C": genC}
                order = ("B", "Bb", "D", "A", "C", "LN")
                nxt_tb = {m: 0 for m in makers}
                active = {m: None for m in makers}
                ln_next = 0
                ln_done = -1
                ln_gen = None
                while True:
                    progressed = False
                    for m in makers:
                        if active[m] is None and nxt_tb[m] < NTB and nxt_tb[m] <= ln_done:
                            active[m] = makers[m](nxt_tb[m], tcols(nxt_tb[m]))
                    if ln_gen is None and ln_next < NTB and min(nxt_tb.values()) >= ln_next - 1:
                        ln_gen = genLN(ln_next, tcols(ln_next))
                    for m in order:
                        if m == "LN":
                            if ln_gen is not None:
                                progressed = True
                                try:
                                    next(ln_gen)
                                except StopIteration:
                                    ln_gen = None
                                    ln_done = ln_next
                                    ln_next += 1
                        elif active[m] is not None:
                            progressed = True
                            try:
                                next(active[m])
                            except StopIteration:
                                active[m] = None
                                nxt_tb[m] += 1
                    if not progressed and all(v >= NTB for v in nxt_tb.values()):
                        break
                    assert progressed or ln_gen is not None or any(nxt_tb[m] <= ln_done for m in makers if nxt_tb[m] < NTB), "scheduler stuck"
                P.end_phase()
            if stop_after == (l, 1):
                break

            with ExitStack() as ph:
                qT = sbt(ph, "qT", [128, S], BF16)
                kT = sbt(ph, "kT", [128, S], BF16)
                vT = sbt(ph, "vT", [128, S], BF16)
                acc = [sbt(ph, "acc%d" % i, [128, S], F32) for i in range(2)]
                vz = [sbt(ph, "vz%d" % i, [128, 256], BF16) for i in range(5)]
                Pm = [sbt(ph, "Pm%d" % i, [128, 256], BF16) for i in range(6)]
                rden = sbt(ph, "rden", [128, TB], F32)
                oC = [sbt(ph, "oC%d" % i, [128, TB], BF16) for i in range(2)]
                for z in vz:
                    memset("pool", z[:, :], 1.0, [z.res])
                pmi = 0
                for hp in (p3test["hps"] if p3test else range(2)):
                    dma("sp", qT[:, :], qkv_d[0][hp * 128:(hp + 1) * 128, :], reads=[R(qkvres, (0, hp))], writes=[qT.res])
                    dma("sp", kT[:, :], qkv_d[1][hp * 128:(hp + 1) * 128, :], reads=[R(qkvres, (1, hp))], writes=[kT.res])
                    dma("sp", vT[:, :], qkv_d[2][hp * 128:(hp + 1) * 128, :], reads=[R(qkvres, (2, hp))], writes=[vT.res])
                    vzi = 0
                    pend = []
                    for d in (p3test["branches"] if p3test else (1, 4, 16)):
                        nb = 32 // d
                        for r in range(d):
                            zprev = None
                            for n in range(nb):
                                cur = slice(r + d * 128 * n, r + d * 128 * n + d * 127 + 1, d)
                                prv = slice(r + d * 128 * (n - 1), r + d * 128 * (n - 1) + d * 127 + 1, d) if n > 0 else None
                                zc = vz[vzi % 5]
                                vzi += 1
                                ptr = PSB()
                                P.op("pe", lambda e, ptr=ptr, cur=cur: e.transpose(out=ptr[:, 0:128], in_=vT[:, cur], identity=identb[:, :]),
                                     reads=[vT.res, identb.res], writes=[ptr.res])
                                cpy("act", zc[:, 0:64], ptr[:, 0:64], [ptr.res], [zc.res])
                                cpy("act", zc[:, 192:256], ptr[:, 64:128], [ptr.res], [zc.res])
                                for hh in range(2):
                                    p0 = 64 * hh
                                    pS = PS()
                                    if n > 0:
                                        mm(pS[:, 0:128], kT[p0:p0 + 64, prv], qT[p0:p0 + 64, cur], True, True, [kT.res, qT.res], [pS.res])
                                    mm(pS[:, 128:256], kT[p0:p0 + 64, cur], qT[p0:p0 + 64, cur], True, True, [kT.res, qT.res], [pS.res])
                                    pm_ = Pm[pmi % len(Pm)]
                                    eng = "dve" if pmi % 2 == 0 else "pool"
                                    pmi += 1
                                    lo = 0 if n > 0 else 128
                                    act(pm_[:, lo:256], pS[:, lo:256], AF.Exp, [pS.res], [pm_.res], scale=0.125)
                                    tt(eng, pm_[:, lo:256], pm_[:, lo:256], maskCb[:, lo:256], ALU.mult, [pm_.res, maskCb.res], [pm_.res])

                                    def stage2(n=n, hh=hh, zprev=zprev, zc=zc, pm_=pm_, cur=cur, d=d):
                                        pO = PS()
                                        if n > 0:
                                            mm(pO[:, 0:128], zprev[:, hh * 128:(hh + 1) * 128], pm_[:, 0:128], True, False, [zprev.res, pm_.res], [pO.res])
                                        mm(pO[:, 0:128], zc[:, hh * 128:(hh + 1) * 128], pm_[:, 128:256], n == 0, True, [zc.res, pm_.res], [pO.res])
                                        if d == (p3test["branches"][0] if p3test else 1):
                                            cpy("dve", acc[hh][:, cur], pO[:, 0:128], [pO.res], [acc[hh].res])
                                        else:
                                            tt("dve", acc[hh][:, cur], acc[hh][:, cur], pO[:, 0:128], ALU.add, [acc[hh].res, pO.res], [acc[hh].res])
                                    pend.append(stage2)
                                    while len(pend) > 3:
                                        pend.pop(0)()
                                zprev = zc
                    while pend:
                        pend.pop(0)()
                    for tb in range(NTB):
                        tc = tcols(tb)
                        pden = PS()
                        mm(pden[:, :], swapLo, acc[0][:, tc], True, False, [cf.res, acc[0].res], [pden.res])
                        mm(pden[:, :], swapHi, acc[1][:, tc], False, True, [cf.res, acc[1].res], [pden.res])
                        recip(rden[:, :], pden[:, :], [pden.res], [rden.res])
                        oc = oC[tb % 2]
                        tt("dve", oc[0:64, :], acc[0][0:64, tc], rden[0:64, :], ALU.mult, [acc[0].res, rden.res], [oc.res])
                        tt("dve", oc[64:128, :], acc[1][64:128, tc], rden[64:128, :], ALU.mult, [acc[1].res, rden.res], [oc.res])
                        dma("sp", oT_d[512 + hp * 128:512 + (hp + 1) * 128, tc], oc[:, :], reads=[oc.res], writes=[R(ores, (4 + hp, tb))])
                P.end_phase()
            if stop_after == (l, 3):
                break

            for f in range(2):
                with ExitStack() as ph:
                    xts = [sbt(ph, "xt%d" % i, [128, 8, TB], F32) for i in range(2)]
                    h2Ts = [sbt(ph, "h2T%d" % i, [128, 8, TB], BF16) for i in range(2)]
                    gTs = [sbt(ph, "gT%d" % i, [128, 11, TB], BF16) for i in range(2)]
                    yb = [[sbt(ph, "yb%d_%d" % (h_, i), [128, TB + 2], F32) for i in range(2)] for h_ in range(2)]
                    halo = sbt(ph, "halo", [128, 22, 2], F32)
                    cc_ = [[sbt(ph, "cc%d_%d" % (h_, i), [128, TB], F32) for i in range(2)] for h_ in range(2)]
                    f_es = [sbt(ph, "f_e%d" % i, [128, TB], F32) for i in range(2)]
                    f_ss = [sbt(ph, "f_s%d" % i, [128, TB], F32) for i in range(2)]
                    if f == 0:
                        wo = sbt(ph, "wo", [128, 8, D], BF16)
                        oTt = sbt(ph, "oTt", [128, 8, TB], BF16)
                        sq = sbt(ph, "sq", [128, TB], BF16)
                        lf = [sbt(ph, "lf%d" % i, [128, TB], F32) for i in range(3)]
                        wor = [Res() for _ in range(8)]
                        for k in range(8):
                            dma("pool", wo[:, k, :], w_out[l][k * 128:(k + 1) * 128, :], writes=[wor[k]])
                    for k in range(8):
                        dma("pool", ws_up[:, k, 0:1408], ffn_up[l][k * 128:(k + 1) * 128, f * 1408:(f + 1) * 1408], writes=[wsr[2 * k]])
                        dma("pool", ws_up[:, k, 1408:2816], ffn_up[l][k * 128:(k + 1) * 128, DFF + f * 1408:DFF + (f + 1) * 1408], writes=[wsr[2 * k + 1]])
                    for jj in range(11):
                        dma("pool", ws_dn[:, jj, :], ffn_down[l][(f * 11 + jj) * 128:(f * 11 + jj + 1) * 128, :], writes=[wsr[16 + jj]])
                    memset("pool", halo[:, :, :], 0.0, [halo.res])

                    def stage_in(tb, part):
                        tc = tcols(tb)
                        xt = xts[tb % 2]
                        h2T = h2Ts[tb % 2]
                        if f == 1:
                            if part == 0:
                                dma("sp", h2T[:, :, :], cp(h2T_d)[:, :, tc], reads=[h2res[tb]], writes=[h2T.res])
                                dma("sp", xt[:, :, :], cp(xT_d)[:, :, tc], reads=[xres[tb]], writes=[xt.res])
                            return
                        if part == 0:
                            dma("sp", xt[:, :, :], cp(xsrc)[:, :, tc], reads=[xres[tb]], writes=[xt.res])
                            dma("sp", oTt[:, :, :], cp(oT_d)[:, :, tc], reads=[R(ores, (c, tb)) for c in range(8)], writes=[oTt.res])
                        elif part == 1:
                            for m in range(8):
                                pm = PS()
                                for kc in range(8):
                                    mm(pm[:, :], wo[:, kc, m * 128:(m + 1) * 128], oTt[:, kc, :], kc == 0, kc == 7, wor + [oTt.res], [pm.res])
                                stt(xt[:, m, :], pm[:, :], g1[:, m:m + 1], xt[:, m, :], ALU.mult, ALU.add, [pm.res, modt.res, xt.res], [xt.res])
                        elif part == 2:
                            pssum = PS()
                            for c in range(8):
                                act(sq[:, :], xt[:, c, :], AF.Square, [xt.res], [sq.res])
                                mm(pssum[:, :], onesb[:, :], sq[:, :], c == 0, c == 7, [onesb.res, sq.res], [pssum.res])
                            act(lf[0][:, :], pssum[:, :], AF.Ln, [pssum.res], [lf[0].res], scale=1.0 / D, bias=EPS)
                            act(lf[1][:, :], lf[0][:, :], AF.Exp, [lf[0].res], [lf[1].res], scale=-0.5)
                        else:
                            for c in range(8):
                                f3 = lf[0] if c % 2 == 0 else lf[2]
                                stt(f3[:, :], xt[:, c, :], gm[:, l, 8 + c:9 + c], lf[1][:, :], ALU.mult, ALU.mult, [xt.res, lf[1].res, gm.res], [f3.res])
                                ts("pool", h2T[:, c, :], f3[:, :], sh2[:, c:c + 1], 1.0, ALU.add, ALU.mult, [f3.res, modt.res], [h2T.res])
                            dma("sp", cp(h2T_d)[:, :, tc], h2T[:, :, :], reads=[h2T.res], writes=[h2res[tb]])

                    pend = []
                    pi = 0
                    for part in range(4):
                        stage_in(0, part)
                    for tb in range(NTB):
                        tc = tcols(tb)
                        xt = xts[tb % 2]
                        h2T = h2Ts[tb % 2]
                        gT = gTs[tb % 2]
                        for jj in range(11):
                            cvs = []
                            for half in range(2):
                                ch = half * 22 + f * 11 + jj
                                hid = half * 11 + jj
                                pz = PS()
                                for k in range(8):
                                    mm(pz[:, :], ws_up[:, k, half * 1408 + jj * 128:half * 1408 + (jj + 1) * 128], h2T[:, k, :], k == 0, k == 7,
                                       wsall + [h2T.res], [pz.res])
                                y = yb[half][pi % 2]
                                cv = cc_[half][pi % 2]
                                cvs.append(cv)
                                cw = o + 136 + ch * 3
                                cpy("pool", y[:, 0:2], halo[:, hid, :], [halo.res], [y.res])
                                cpy("act", y[:, 2:TB + 2], pz[:, :], [pz.res], [y.res])
                                act(cv[:, :], pz[:, :], AF.Identity, [pz.res, pp.res], [cv.res],
                                    scale=pp[:, cw + 2:cw + 3], bias=pp[:, o + 268 + ch:o + 269 + ch])
                                cpy("pool", halo[:, hid, :], y[:, TB:TB + 2], [y.res], [halo.res])
                                stt(cv[:, :], y[:, 1:TB + 1], pp[:, cw + 1:cw + 2], cv[:, :], ALU.mult, ALU.add, [y.res, pp.res, cv.res], [cv.res])
                                stt(cv[:, :], y[:, 0:TB], pp[:, cw:cw + 1], cv[:, :], ALU.mult, ALU.add, [y.res, pp.res, cv.res], [cv.res])
                            f_e = f_es[pi % 2]
                            f_s = f_ss[pi % 2]
                            pi += 1
                            act(f_e[:, :], cvs[0][:, :], AF.Exp, [cvs[0].res], [f_e.res], scale=-1.0)
                            act(f_e[:, :], f_e[:, :], AF.Ln, [f_e.res], [f_e.res], bias=1.0)
                            act(f_e[:, :], f_e[:, :], AF.Exp, [f_e.res], [f_e.res], scale=-1.0)
                            tt("dve", f_s[:, :], cvs[0][:, :], f_e[:, :], ALU.mult, [cvs[0].res, f_e.res], [f_s.res])
                            tt("dve", gT[:, jj, :], f_s[:, :], cvs[1][:, :], ALU.mult, [f_s.res, cvs[1].res], [gT.res])
                            if jj == 2:
                                while pend:
                                    pend.pop(0)()
                            if tb + 1 < NTB and jj in (2, 5, 7, 9):
                                stage_in(tb + 1, {2: 0, 5: 1, 7: 2, 9: 3}[jj])

                        def down(tb=tb, tc=tc, xt=xt, gT=gT):
                            for m in range(8):
                                pd = PS()
                                for jj in range(11):
                                    mm(pd[:, :], ws_dn[:, jj, m * 128:(m + 1) * 128], gT[:, jj, :], jj == 0, jj == 10, wsall + [gT.res], [pd.res])
                                stt(xt[:, m, :], pd[:, :], g2[:, m:m + 1], xt[:, m, :], ALU.mult, ALU.add, [pd.res, modt.res, xt.res], [xt.res])
                            dma("sp", cp(xT_d)[:, :, tc], xt[:, :, :], reads=[xt.res], writes=[xres[tb]])
                        pend.append(down)
                    while pend:
                        pend.pop(0)()
                    P.end_phase()

        if stop_after is None and final:
            with ExitStack() as ph:
                xt = sbt(ph, "xt", [128, 8, TB], F32)
                ot = sbt(ph, "ot", [128, 8, TB], F32)
                sq = sbt(ph, "sq", [128, TB], BF16)
                lf = [sbt(ph, "lf%d" % i, [128, TB], F32) for i in range(3)]
                xfin = xT_in if nlayers == 0 else xT_d
                for tb in range(NTB):
                    tc = tcols(tb)
                    dma("sp", xt[:, :, :], cp(xfin)[:, :, tc], reads=[xres[tb]], writes=[xt.res])
                    rmsnorm_fm(xt, pp[:, L * PPL:L * PPL + 8], None, ot, True, sq, lf[0], lf[1], lf[2])
                    dma("sp", cp(outT)[:, :, tc], ot[:, :, :], reads=[ot.res], writes=[Res()])
                P.end_phase()
        print("total ops", P.total_ops, "max sem count/phase", P.maxcnt, P.dcnt)
    return nc


_CACHE = {}
NSPLIT = 1


def kernel(**inputs):
    inp = {k: np.ascontiguousarray(np.asarray(v, dtype=np.float32)) for k, v in inputs.items()}
    pp = _pack_pp(inp)
    bc = _pack_bc(inp)
    cf, cs = _consts()
    B = inp["x"].shape[0]
    per = L // NSPLIT
    xTs = [np.ascontiguousarray(inp["x"][b].T) for b in range(B)]
    for s in range(NSPLIT):
        last = (s == NSPLIT - 1)
        key = ("prog", s, NSPLIT)
        if key not in _CACHE:
            _CACHE[key] = build_program(nlayers=per, l0=s * per, final=last)
        nc = _CACHE[key]
        in_maps = []
        for b in range(B):
            in_maps.append({
                "xT": xTs[b],
                "cT": np.ascontiguousarray(inp["c"][b].reshape(8, 128).T),
                "ada_w": inp["ada_w"], "w_in": inp["w_in"], "w_out": inp["w_out"],
                "ffn_up": inp["ffn_up"], "ffn_down": inp["ffn_down"],
                "gla_w2": inp["gla_w2"], "sgu_w": inp["sgu_w"],
                "pp": pp, "bc": bc, "cf": cf, "cs": cs,
            })
        res = run_bass_kernel_spmd(nc, in_maps, core_ids=list(range(B)))
        if last:
            out = np.stack([np.ascontiguousarray(r["outT"].T) for r in res.results], axis=0)
        else:
            xTs = [np.ascontiguousarray(np.asarray(r["xT_d"], dtype=np.float32)) for r in res.results]
    return out.astype(np.float32)
```

```python
import os
import math
import numpy as np
from contextlib import ExitStack
import concourse.bass as bass
import concourse.mybir as mybir
from concourse.bass_utils import run_bass_kernel_spmd

F32 = mybir.dt.float32
BF16 = mybir.dt.bfloat16
ALU = mybir.AluOpType
AF = mybir.ActivationFunctionType
AX = mybir.AxisListType

L = 4
S = 4096
D = 1024
TB = 512
NTB = S // TB
NIN = 2576
NINX = NIN + 512
DFF = 2816
EPS = 1e-6
PPL = 312
NPP = L * PPL + 8
NBC = 896
NCF = 1156
GELU_K = 2.0 * math.sqrt(2.0 / math.pi)


class Res:
    __slots__ = ("w", "r")

    def __init__(self):
        self.w = None
        self.r = []


class Op:
    __slots__ = ("eng", "fn", "deps", "dma", "sig", "sigval", "sem", "semval", "semprev", "phase", "persist", "seq")

    def __init__(self, eng, fn, dma, phase, persist):
        self.eng = eng
        self.fn = fn
        self.dma = dma
        self.deps = []
        self.sig = False
        self.sigval = 0
        self.sem = None
        self.semval = 0
        self.semprev = 0
        self.phase = phase
        self.persist = persist
        self.seq = 0


class Tile:
    __slots__ = ("t", "res")

    def __init__(self, t):
        self.t = t
        self.res = Res()

    def __getitem__(self, k):
        return self.t[k]


class Prog:
    ENGS = ("pe", "act", "dve", "pool", "sp")
    CENGS = ("pe", "act", "dve", "pool")

    def __init__(self, nc, stack, n_dma=(("sp", 24), ("pool", 12))):
        self.nc = nc
        self.phase = 0
        self.ops = []
        self.esets = [{e: stack.enter_context(nc.semaphore("s%d_%s" % (i, e))) for e in self.CENGS} for i in range(3)]
        self.seqn = 0
        self.dsem = {q: [stack.enter_context(nc.semaphore("d_%s%d" % (q, i))) for i in range(n)] for q, n in n_dma}
        self.cnt = {e: 0 for e in self.CENGS}
        self.dcnt = {q: 0 for q in self.dsem}
        self.dval = {q: [0] * len(self.dsem[q]) for q in self.dsem}
        self.pending_persist = []
        self.total_ops = 0

    def op(self, eng, fn, reads=(), writes=(), dma=False, persist=False):
        o = Op(eng, fn, dma, self.phase, persist)
        self.seqn += 1
        o.seq = self.seqn
        deps = {}
        for r in reads:
            if r.w is not None:
                deps[id(r.w)] = r.w
        for w in writes:
            if w.w is not None:
                deps[id(w.w)] = w.w
            for q in w.r:
                deps[id(q)] = q
        for r in reads:
            if not dma:
                r.r = [q for q in r.r if q.dma or q.eng != eng]
            r.r.append(o)
        for w in writes:
            w.w = o
            w.r = []
        dl = []
        best = {}
        for d in deps.values():
            if d.phase != self.phase:
                if d.dma and d.persist:
                    dl.append(d)
                continue
            if d.dma:
                dl.append(d)
                continue
            if d.eng == "pe" and eng == "pe" and not dma:
                continue
            b = best.get(d.eng)
            if b is None or d.seq > b.seq:
                best[d.eng] = d
        dl.extend(best.values())
        o.deps = dl
        self.ops.append(o)
        return o

    def end_phase(self):
        nc = self.nc
        ops = self.ops
        self.total_ops += len(ops)
        self.esem = self.esets[self.phase % 3]
        nxt = self.esets[(self.phase + 1) % 3]
        self.cnt = {e: 0 for e in self.CENGS}
        for o in ops:
            for d in o.deps:
                if not d.dma:
                    d.sig = True
        streams = {e: [] for e in self.ENGS}
        for o in ops:
            streams[o.eng].append(o)
        last = {}
        for e in self.CENGS:
            for o in reversed(streams[e]):
                if not o.dma:
                    o.sig = True
                    last[e] = o
                    break
        for o in ops:
            if o.dma:
                q = o.eng
                i = self.dcnt[q] % len(self.dsem[q])
                self.dcnt[q] += 1
                o.sem = self.dsem[q][i]
                o.semprev = self.dval[q][i]
                self.dval[q][i] += 16
                o.semval = self.dval[q][i]
            elif o.sig:
                self.cnt[o.eng] += 1
                o.sigval = self.cnt[o.eng]
        bar = {}
        for o in ops:
            if o.dma and not o.persist:
                bar[id(o.sem)] = (o.sem, o.semval)
        for e, o in last.items():
            bar[id(self.esem[e])] = (self.esem[e], o.sigval)
        esem = self.esem

        def run(ename, eng):
            waited = {}
            if ename in nxt:
                eng.sem_clear(nxt[ename])
            for o in streams[ename]:
                if o.dma and o.semprev > 0:
                    k = id(o.sem)
                    if waited.get(k, 0) < o.semprev:
                        eng.wait_ge(o.sem, o.semprev)
                        waited[k] = o.semprev
                for d in o.deps:
                    if d.dma:
                        s, v = d.sem, d.semval
                    else:
                        s, v = esem[d.eng], d.sigval
                    k = id(s)
                    if waited.get(k, 0) < v:
                        eng.wait_ge(s, v)
                        waited[k] = v
                ins = o.fn(eng)
                if o.dma:
                    ins.then_inc(o.sem, 16)
                elif o.sig:
                    ins.then_inc(esem[ename], 1)
            for s, v in bar.values():
                if waited.get(id(s), 0) < v:
                    eng.wait_ge(s, v)

        with nc.Block() as block:
            @block.tensor
            def _(eng):
                run("pe", eng)

            @block.scalar
            def _(eng):
                run("act", eng)

            @block.vector
            def _(eng):
                run("dve", eng)

            @block.gpsimd
            def _(eng):
                run("pool", eng)

            @block.sync
            def _(eng):
                run("sp", eng)
        self.maxcnt = max(getattr(self, 'maxcnt', 0), max(self.cnt.values()))
        assert max(self.cnt.values()) < 3000, self.cnt
        self.ops = []
        self.phase += 1

    def final_wait_persist(self):
        return [(o.sem, o.semval) for o in self.pending_persist]


def _pack_pp(inp):
    pp = np.zeros((128, NPP), np.float32)
    for l in range(L):
        o = l * PPL
        pp[:, o + 0:o + 48] = inp["ada_b"][l].reshape(48, 128).T
        pp[:, o + 48:o + 56] = inp["ln1_g"][l].reshape(8, 128).T
        pp[:, o + 56:o + 64] = inp["ln2_g"][l].reshape(8, 128).T
        pp[:, o + 64:o + 126] = inp["conv_w"][l].reshape(31, 2, 128).transpose(2, 1, 0).reshape(128, 62)
        pp[:, o + 126:o + 128] = inp["conv_b"][l].reshape(2, 128).T
        pp[:, o + 128:o + 130] = inp["cln_g"][l].reshape(2, 128).T
        pp[:, o + 130:o + 132] = inp["cln_b"][l].reshape(2, 128).T
        pp[:, o + 132:o + 136] = inp["sgu_b"][l].T
        pp[:, o + 136:o + 268] = inp["ffn_conv_w"][l].reshape(3, 44, 128).transpose(2, 1, 0).reshape(128, 132)
        pp[:, o + 268:o + 312] = inp["ffn_conv_b"][l].reshape(44, 128).T
    pp[:, L * PPL:L * PPL + 8] = inp["lnf_g"].reshape(8, 128).T
    return pp


def _pack_bc(inp):
    bc = np.zeros((L, 128, NBC), np.float32)
    for l in range(L):
        row = np.concatenate([inp["gla_b"][l], np.tile(inp["gla_norm_g"][l], 4), inp["sgu_ln_g"][l], inp["sgu_ln_b"][l]])
        bc[l] = np.broadcast_to(row[None, :], (128, NBC))
    return bc


def _consts():
    cf = np.zeros((128, NCF), np.float32)
    i = np.arange(128)
    cf[:, 0:128] = np.eye(128)
    cf[:, 128:256] = (i[:, None] <= i[None, :]) / 16.0
    cf[:, 256:384] = (i[:, None] > i[None, :]) / 16.0
    cf[:, 384:512] = (i[:, None] <= i[None, :])
    cf[:, 512:640] = (i[:, None] >= i[None, :])
    cf[:, 640:896] = ((i[:, None] // 32) == (np.arange(256)[None, :] // 64))
    cf[:, 896:1024] = ((i[None, :] < 64) & (i[:, None] == i[None, :] + 64))
    cf[:, 1024:1152] = ((i[None, :] >= 64) & (i[:, None] == i[None, :] - 64))
    cf[:, 1152:1156] = ((i[:, None] // 32) == np.arange(4)[None, :])
    inv = (1.0 / (np.float32(10000.0) ** (np.arange(0, 64, 2, dtype=np.float32) / np.float32(64)))).astype(np.float32)
    ang = np.arange(S, dtype=np.float32)[:, None] * inv[None, :]
    cs = np.zeros((2, 128, S), np.float32)
    f = i % 32
    cs[0] = np.cos(ang).astype(np.float32).T[f]
    cs[1] = np.sin(ang).astype(np.float32).T[f]
    return cf, cs


def build_program(nlayers=L, debug=False, stop_after=None, p3test=None, l0=0, final=True):
    nc = bass.Bass("TRN2", target_bir_lowering=False)
    EI = "ExternalInput"
    xT_in = nc.dram_tensor("xT", [D, S], F32, kind=EI).ap()
    cT_in = nc.dram_tensor("cT", [128, 8], F32, kind=EI).ap()
    ada_w = nc.dram_tensor("ada_w", [L, D, 6 * D], F32, kind=EI).ap()
    w_in = nc.dram_tensor("w_in", [L, D, NIN], F32, kind=EI).ap()
    w_out = nc.dram_tensor("w_out", [L, D, D], F32, kind=EI).ap()
    ffn_up = nc.dram_tensor("ffn_up", [L, D, 2 * DFF], F32, kind=EI).ap()
    ffn_down = nc.dram_tensor("ffn_down", [L, DFF, D], F32, kind=EI).ap()
    gla_w2 = nc.dram_tensor("gla_w2", [L, 16, 128], F32, kind=EI).ap()
    sgu_w = nc.dram_tensor("sgu_w", [L, 4, 128, 128], F32, kind=EI).ap()
    pp_in = nc.dram_tensor("pp", [128, NPP], F32, kind=EI).ap()
    bc_in = nc.dram_tensor("bc", [L, 128, NBC], F32, kind=EI).ap()
    cf_in = nc.dram_tensor("cf", [128, NCF], F32, kind=EI).ap()
    cs_in = nc.dram_tensor("cs", [2, 128, S], F32, kind=EI).ap()
    outT = nc.dram_tensor("outT", [D, S], F32, kind="ExternalOutput").ap() if final else None
    SK = "ExternalOutput" if debug else "Internal"
    xT_d = nc.dram_tensor("xT_d", [D, S], F32, kind=(SK if final else "ExternalOutput")).ap()
    oT_d = nc.dram_tensor("oT_d", [D, S], BF16, kind=SK).ap()
    qkv_d = nc.dram_tensor("qkv_d", [3, 256, S], BF16, kind=(EI if p3test else SK)).ap()
    h2T_d = nc.dram_tensor("h2T_d", [D, S], BF16, kind=SK).ap()

    def cp(ap):
        return ap.rearrange("(c p) t -> p c t", p=128)

    with ExitStack() as st:
        P = Prog(nc, st)

        uid = [0]

        def sbt(stack, name, shape, dt):
            uid[0] += 1
            return Tile(stack.enter_context(nc.sbuf_tensor("sb%d_%s" % (uid[0], name), shape, dt)))

        def pst(stack, name, shape, dt):
            return Tile(stack.enter_context(nc.psum_tensor("ps_" + name, shape, dt)))

        cf = sbt(st, "cf", [128, NCF], F32)
        pp = sbt(st, "pp", [128, NPP], F32)
        modt = sbt(st, "mod", [128, L, 48], F32)
        gm = sbt(st, "gm", [128, L, 16], F32)
        identb = sbt(st, "identb", [128, 128], BF16)
        onesb = sbt(st, "onesb", [128, 128], BF16)
        onesf = sbt(st, "onesf", [128, 128], F32)
        maskCb = sbt(st, "maskCb", [128, 256], BF16)
        maskLE4 = sbt(st, "maskLE4", [128, 512], F32)
        ws = sbt(st, "ws", [128, 34560], BF16)
        psf = [pst(st, "psf%d" % i, [128, 512], F32) for i in range(6)]
        psb = [pst(st, "psb%d" % i, [128, 1024], BF16) for i in range(2)]
        psi = [0, 0]

        def PS():
            t = psf[psi[0] % len(psf)]
            psi[0] += 1
            return t

        def PSB():
            t = psb[psi[1] % len(psb)]
            psi[1] += 1
            return t

        ws_in = ws[:, 0:8 * NINX].rearrange("p (k n) -> p k n", k=8)
        ws_out = ws[:, 8 * NINX:8 * NINX + 8192].rearrange("p (k n) -> p k n", k=8)
        ws_up = ws[:, 0:22528].rearrange("p (k n) -> p k n", k=8)
        ws_dn = ws[:, 22528:33792].rearrange("p (j n) -> p j n", j=11)

        ident = cf[:, 0:128]
        triT = cf[:, 128:256]
        triS = cf[:, 256:384]
        maskLE = cf[:, 384:512]
        blockmask = cf[:, 640:896]
        swapLo = cf[:, 896:1024]
        swapHi = cf[:, 1024:1152]
        headmask = cf[:, 1152:1156]

        wsr = [Res() for _ in range(40)]
        wsrot = Res()
        wsall = wsr + [wsrot]
        xres = [Res() for _ in range(NTB)]
        ores = {}
        qkvres = {}
        h2res = [Res() for _ in range(NTB)]

        def R(dct, key):
            if key not in dct:
                dct[key] = Res()
            return dct[key]

        def tcols(tb):
            return slice(tb * TB, (tb + 1) * TB)

        def act(out, in_, func, reads, writes, scale=1.0, bias=0.0):
            P.op("act", lambda e: e.activation(out=out, in_=in_, func=func, scale=scale, bias=bias), reads=reads, writes=writes)

        def dma(q, out, in_, reads=(), writes=(), persist=False):
            P.op(q, lambda e: e.dma_start(out=out, in_=in_), reads=reads, writes=writes, dma=True, persist=persist)

        def mm(out, lhsT, rhs, start, stop, reads, writes):
            P.op("pe", lambda e: e.matmul(out, lhsT=lhsT, rhs=rhs, start=start, stop=stop), reads=reads, writes=writes)

        def tt(eng, out, in0, in1, op, reads, writes):
            P.op(eng, lambda e: e.tensor_tensor(out=out, in0=in0, in1=in1, op=op), reads=reads, writes=writes)

        def ts(eng, out, in0, s1, s2, op0, op1, reads, writes):
            P.op(eng, lambda e: e.tensor_scalar(out=out, in0=in0, scalar1=s1, scalar2=s2, op0=op0, op1=op1), reads=reads, writes=writes)

        def stt(out, in0, scalar, in1, op0, op1, reads, writes):
            P.op("dve", lambda e: e.scalar_tensor_tensor(out=out, in0=in0, scalar=scalar, in1=in1, op0=op0, op1=op1), reads=reads, writes=writes)

        def recip(out, in_, reads, writes):
            P.op("dve", lambda e: e.reciprocal(out=out, in_=in_), reads=reads, writes=writes)

        def cpy(eng, out, in_, reads, writes):
            if eng == "act":
                act(out, in_, AF.Copy, reads, writes)
            else:
                P.op(eng, lambda e: e.tensor_copy(out=out, in_=in_), reads=reads, writes=writes)

        def memset(eng, ap, val, writes):
            P.op(eng, lambda e: e.memset(ap, val), writes=writes)

        def rmsnorm_fm(xt, gmcol, shcol, outT_tile, out_dt_f32, sq, f1, f2, f3):
            pssum = PS()
            for c in range(8):
                act(sq[:, :], xt[:, c, :], AF.Square, [xt.res], [sq.res])
                mm(pssum[:, :], onesb[:, :], sq[:, :], c == 0, c == 7, [onesb.res, sq.res], [pssum.res])
            act(f1[:, :], pssum[:, :], AF.Ln, [pssum.res], [f1.res], scale=1.0 / D, bias=EPS)
            act(f2[:, :], f1[:, :], AF.Exp, [f1.res], [f2.res], scale=-0.5)
            for c in range(8):
                if shcol is None:
                    stt(outT_tile[:, c, :], xt[:, c, :], gmcol[:, c:c + 1], f2[:, :], ALU.mult, ALU.mult,
                        [xt.res, f2.res, pp.res, gm.res], [outT_tile.res])
                else:
                    stt(f3[:, :], xt[:, c, :], gmcol[:, c:c + 1], f2[:, :], ALU.mult, ALU.mult,
                        [xt.res, f2.res, pp.res, gm.res], [f3.res])
                    ts("pool", outT_tile[:, c, :], f3[:, :], shcol[:, c:c + 1], 1.0, ALU.add, ALU.mult,
                       [f3.res, modt.res], [outT_tile.res])

        with ExitStack() as ph:
            c_sb = sbt(ph, "c_sb", [128, 8], F32)
            t8a = sbt(ph, "t8a", [128, 8], F32)
            t8b = sbt(ph, "t8b", [128, 8], F32)
            cond = sbt(ph, "cond", [128, 8], F32)
            aw = [sbt(ph, "aw%d" % i, [128, 8, 512], BF16) for i in range(3)]
            condb = sbt(ph, "condb", [128, 8], BF16)
            modrow = sbt(ph, "modrow", [1, 6 * D], F32)
            dma("sp", cf[:, :], cf_in, writes=[cf.res])
            dma("sp", pp[:, :], pp_in, writes=[pp.res])
            dma("sp", c_sb[:, :], cT_in, writes=[c_sb.res])
            cpy("dve", identb[:, :], ident, [cf.res], [identb.res])
            memset("dve", onesb[:, :], 1.0, [onesb.res])
            memset("pool", onesf[:, :], 1.0, [onesf.res])
            cpy("dve", maskCb[:, 0:128], cf[:, 512:640], [cf.res], [maskCb.res])
            cpy("dve", maskCb[:, 128:256], cf[:, 384:512], [cf.res], [maskCb.res])
            for h in range(4):
                cpy("pool", maskLE4[:, h * 128:(h + 1) * 128], maskLE, [cf.res], [maskLE4.res])
            act(t8a[:, :], c_sb[:, :], AF.Exp, [c_sb.res], [t8a.res], scale=-1.0)
            ts("dve", t8a[:, :], t8a[:, :], 1.0, None, ALU.add, ALU.bypass, [t8a.res], [t8a.res])
            recip(t8b[:, :], t8a[:, :], [t8a.res], [t8b.res])
            tt("dve", cond[:, :], c_sb[:, :], t8b[:, :], ALU.mult, [c_sb.res, t8b.res], [cond.res])
            cpy("dve", condb[:, :], cond[:, :], [cond.res], [condb.res])
            awi = 0
            for l in (range(0) if p3test else range(l0, l0 + nlayers)):
                for n in range(12):
                    a = aw[awi % 3]
                    awi += 1
                    dma("pool", a[:, :, :], ada_w[l][:, n * 512:(n + 1) * 512].rearrange("(k p) n -> p k n", p=128), writes=[a.res])
                    pr = PS()
                    for k in range(8):
                        mm(pr[0:1, 0:512], condb[:, k:k + 1], a[:, k, :], k == 0, k == 7, [condb.res, a.res], [pr.res])
                    cpy("act", modrow[0:1, n * 512:(n + 1) * 512], pr[0:1, 0:512], [pr.res], [modrow.res])
                pm = PS()
                for m in range(48):
                    mm(pm[:, m:m + 1], modrow[0:1, m * 128:(m + 1) * 128], cf[0:1, 0:1], True, True, [modrow.res, cf.res], [pm.res])
                o = l * PPL
                tt("dve", modt[:, l, :], pm[:, 0:48], pp[:, o:o + 48], ALU.add, [pm.res, pp.res], [modt.res])
                stt(gm[:, l, 0:8], modt[:, l, 8:16], 1.0, pp[:, o + 48:o + 56], ALU.add, ALU.mult, [modt.res, pp.res], [gm.res])
                stt(gm[:, l, 8:16], modt[:, l, 32:40], 1.0, pp[:, o + 56:o + 64], ALU.add, ALU.mult, [modt.res, pp.res], [gm.res])
            P.end_phase()

        for l in range(l0, l0 + nlayers):
            o = l * PPL
            xsrc = xT_in if l == l0 else xT_d
            sh1 = modt[:, l, 0:8]
            g1 = modt[:, l, 16:24]
            sh2 = modt[:, l, 24:32]
            g2 = modt[:, l, 40:48]
            with ExitStack() as ph:
              if not p3test:
                diag = sbt(ph, "diag", [128, 2, 31, 128], BF16)
                xt = sbt(ph, "xt", [128, 8, TB], F32)
                hTs = [sbt(ph, "hT%d" % i, [128, 8, TB], BF16) for i in range(2)]
                hA = [sbt(ph, "hA%d" % i, [128, 2, TB + 30], BF16) for i in range(2)]
                bcs = sbt(ph, "bcs", [128, NBC], F32)
                w2s = sbt(ph, "w2s", [16, 128], F32)
                sgWT = sbt(ph, "sgWT", [128, 4, 128], BF16)
                Sfull = sbt(ph, "Sfull", [128, 256], F32)
                Sm = [sbt(ph, "Sm%d" % i, [128, 256], BF16) for i in range(2)]
                sq = sbt(ph, "sq", [128, TB], BF16)
                lf = [sbt(ph, "lf%d" % i, [128, TB], F32) for i in range(3)]
                a_e = sbt(ph, "a_e", [128, TB], F32)
                yA = [sbt(ph, "yA%d" % i, [128, TB], F32) for i in range(2)]
                a_yb = sbt(ph, "a_yb", [128, TB], BF16)
                a_sb = sbt(ph, "a_sb", [128, TB], BF16)
                a_m = sbt(ph, "a_m", [128, TB], F32)
                a_m2 = sbt(ph, "a_m2", [128, TB], F32)
                a_rs = sbt(ph, "a_rs", [128, TB], F32)
                a_d = sbt(ph, "a_d", [128, TB], F32)
                a_o = sbt(ph, "a_o", [128, TB], BF16)
                lrs = sbt(ph, "lrs", [16, TB], F32)
                qbs = sbt(ph, "qbs", [128, TB], F32)
                kbs = sbt(ph, "kbs", [128, TB], F32)
                b_e1 = sbt(ph, "b_e1", [128, 128], F32)
                b_sp = sbt(ph, "b_sp", [128, 128], F32)
                b_E2 = [sbt(ph, "b_E%d" % i, [128, 384], F32) for i in range(2)]
                b_qt2 = [sbt(ph, "b_qt%d" % i, [128, 128], BF16) for i in range(2)]
                b_kt = sbt(ph, "b_kt", [128, 128], BF16)
                b_k42 = [sbt(ph, "b_k4%d" % i, [128, 512], BF16) for i in range(2)]
                b_ke2 = [sbt(ph, "b_ke%d" % i, [128, 128], BF16) for i in range(2)]
                b_kf = sbt(ph, "b_kf", [128, 128], F32)
                b_v2 = [sbt(ph, "b_v%d" % i, [128, 256], BF16) for i in range(2)]
                b_at = sbt(ph, "b_at", [128, 512], BF16)
                b_osq = sbt(ph, "b_osq", [128, 256], F32)
                b_s4 = sbt(ph, "b_s4", [128, 12], F32)
                b_eg = sbt(ph, "b_eg", [128, 256], F32)
                b_gt = sbt(ph, "b_gt", [128, 256], F32)
                b_ob = sbt(ph, "b_ob", [128, 256], BF16)
                oBT = sbt(ph, "oBT", [128, 2, TB], BF16)
                d_xs = sbt(ph, "d_xs", [128, 512], F32)
                d_a = sbt(ph, "d_a", [128, 512], F32)
                d_b = sbt(ph, "d_b", [128, 512], F32)
                d_ge = sbt(ph, "d_ge", [128, 512], F32)
                d_st = sbt(ph, "d_st", [128, 16], F32)
                d_vn = sbt(ph, "d_vn", [128, 256], F32)
                d_vl = sbt(ph, "d_vl", [128, 256], BF16)
                d_od = sbt(ph, "d_od", [128, 256], BF16)
                oDT = sbt(ph, "oDT", [128, 2, TB], BF16)
                csc = sbt(ph, "csc", [128, TB], F32)
                css = sbt(ph, "css", [128, TB], F32)
                c_t1 = sbt(ph, "c_t1", [128, TB], F32)
                c_t2 = sbt(ph, "c_t2", [128, TB], F32)
                c_o = [sbt(ph, "c_o%d" % i, [128, TB], BF16) for i in range(2)]

                for k in range(8):
                    dma("pool", ws_in[:, k, 0:NIN], w_in[l][k * 128:(k + 1) * 128, :], writes=[wsr[k]])
                dma("sp", bcs[:, :], bc_in[l], writes=[bcs.res])
                dma("sp", w2s[:, :], gla_w2[l], writes=[w2s.res])
                dma("sp", d_xs[:, :].rearrange("p (g s) -> p g s", g=4), sgu_w[l].rearrange("g t s -> t g s"), writes=[d_xs.res])
                for qi, c0 in enumerate((1296, 1552)):
                    r0 = NIN + qi * 256
                    for k in range(8):
                        src = ws_in[:, k, c0:c0 + 256].rearrange("p (h t r) -> p h t r", h=4, t=2)
                        dst = ws_in[:, k, r0:r0 + 256].rearrange("p (h t r) -> p h t r", h=4, t=2)
                        ts("dve" if k % 2 == 0 else "pool", dst[:, :, 0, :], src[:, :, 1, :], -1.0, 1.0, ALU.mult, ALU.mult, [wsr[k]], [wsrot])
                        cpy("pool" if k % 2 == 0 else "dve", dst[:, :, 1, :], src[:, :, 0, :], [wsr[k]], [wsrot])
                for c in range(2):
                    for k in range(31):
                        ts("dve" if (k % 2 == 0) else "pool", diag[:, c, k, :], ident, pp[:, o + 64 + c * 31 + k:o + 64 + c * 31 + k + 1], 1.0,
                           ALU.mult, ALU.mult, [cf.res, pp.res], [diag.res])
                psg = PS()
                for g in range(4):
                    P.op("pe", lambda e, g=g: e.transpose(out=psg[:, g * 128:(g + 1) * 128], in_=d_xs[:, g * 128:(g + 1) * 128], identity=ident),
                         reads=[d_xs.res, cf.res], writes=[psg.res])
                tt("dve", sgWT[:, :, :].rearrange("p g t -> p (g t)"), psg[:, :], maskLE4[:, :], ALU.mult, [psg.res, maskLE4.res], [sgWT.res])
                memset("dve", Sfull[:, :], 0.0, [Sfull.res])
                memset("pool", Sm[0][:, :], 0.0, [Sm[0].res])
                memset("pool", hA[0][:, :, 0:30], 0.0, [hA[0].res])
                memset("pool", hA[1][:, :, 0:30], 0.0, [hA[1].res])

                def fm(hT, c0, M=128):
                    pt = PS()
                    for k in range(8):
                        mm(pt[0:M, :], ws_in[:, k, c0:c0 + M], hT[:, k, :], k == 0, k == 7, wsall + [hT.res], [pt.res])
                    return pt

                def tm(hT, tt_, c0, N):
                    pt = PS()
                    for k in range(8):
                        mm(pt[:, 0:N], hT[:, k, tt_ * 128:(tt_ + 1) * 128], ws_in[:, k, c0:c0 + N], k == 0, k == 7, wsall + [hT.res], [pt.res])
                    return pt

                schunk = [0]

                def sigm(t, src, reads):
                    act(t[:, :], src, AF.Exp, reads, [t.res], scale=-1.0)
                    act(t[:, :], t[:, :], AF.Ln, [t.res], [t.res], bias=1.0)
                    act(t[:, :], t[:, :], AF.Exp, [t.res], [t.res], scale=-1.0)

                def genA(tb, tc):
                    hT = hTs[tb % 2]
                    hcur = hA[tb % 2]
                    hprev = hA[(tb + 1) % 2]
                    if tb > 0:
                        cpy("pool", hcur[:, :, 0:30], hprev[:, :, TB:TB + 30], [hprev.res], [hcur.res])
                    for c in range(2):
                        pv = fm(hT, c * 128)
                        pg = fm(hT, 256 + c * 128)
                        sigm(a_e, pg[:, :], [pg.res])
                        tt("dve", hcur[:, c, 30:30 + TB], pv[:, :], a_e[:, :], ALU.mult, [pv.res, a_e.res], [hcur.res])
                        yield
                        py = PS()
                        for k in range(31):
                            mm(py[:, :], diag[:, c, k, :], hcur[:, c, k:k + TB], k == 0, k == 30, [diag.res, hcur.res], [py.res])
                        act(yA[c][:, :], py[:, :], AF.Identity, [py.res, pp.res], [yA[c].res], bias=pp[:, o + 126 + c:o + 127 + c])
                        yield
                    pmean = PS()
                    pmsq = PS()
                    for c in range(2):
                        cpy("dve", a_yb[:, :], yA[c][:, :], [yA[c].res], [a_yb.res])
                        act(a_sb[:, :], yA[c][:, :], AF.Square, [yA[c].res], [a_sb.res])
                        mm(pmean[:, :], onesb[:, :], a_yb[:, :], c == 0, c == 1, [onesb.res, a_yb.res], [pmean.res])
                        mm(pmsq[:, :], onesb[:, :], a_sb[:, :], c == 0, c == 1, [onesb.res, a_sb.res], [pmsq.res])
                    act(a_m[:, :], pmean[:, :], AF.Copy, [pmean.res], [a_m.res], scale=1.0 / 256)
                    tt("dve", a_m2[:, :], a_m[:, :], a_m[:, :], ALU.mult, [a_m.res], [a_m2.res])
                    stt(a_m2[:, :], pmsq[:, :], 1.0 / 256, a_m2[:, :], ALU.mult, ALU.subtract, [pmsq.res, a_m2.res], [a_m2.res])
                    act(a_rs[:, :], a_m2[:, :], AF.Ln, [a_m2.res], [a_rs.res], bias=EPS)
                    act(a_rs[:, :], a_rs[:, :], AF.Exp, [a_rs.res], [a_rs.res], scale=-0.5)
                    yield
                    for c in range(2):
                        tt("dve", a_d[:, :], yA[c][:, :], a_m[:, :], ALU.subtract, [yA[c].res, a_m.res], [a_d.res])
                        tt("dve", a_d[:, :], a_d[:, :], a_rs[:, :], ALU.mult, [a_d.res, a_rs.res], [a_d.res])
                        act(a_d[:, :], a_d[:, :], AF.Identity, [a_d.res, pp.res], [a_d.res],
                            scale=pp[:, o + 128 + c:o + 129 + c], bias=pp[:, o + 130 + c:o + 131 + c])
                        sigm(a_e, a_d[:, :], [a_d.res])
                        tt("dve", a_o[:, :], a_d[:, :], a_e[:, :], ALU.mult, [a_d.res, a_e.res], [a_o.res])
                        dma("sp", oT_d[c * 128:(c + 1) * 128, tc], a_o[:, :], reads=[a_o.res], writes=[R(ores, (c, tb))])
                        yield

                bfront_done = {}
                bback_done = {}

                def genB(tb, tc):
                    hT = hTs[tb % 2]
                    pq = fm(hT, 512)
                    cpy("act", qbs[:, :], pq[:, :], [pq.res], [qbs.res])
                    pk = fm(hT, 640)
                    cpy("act", kbs[:, :], pk[:, :], [pk.res], [kbs.res])
                    plr = fm(hT, 1280, 16)
                    cpy("act", lrs[:, :], plr[0:16, :], [plr.res], [lrs.res])
                    yield
                    for t4 in range(4):
                        t0 = t4 * 128
                        tsl = slice(t0, t0 + 128)
                        i2 = t4 % 2
                        gidx = tb * 4 + t4
                        while gidx >= 2 and (gidx - 2) not in bback_done:
                            yield
                        pkv = tm(hT, t4, 640, 384)
                        cpy("act", b_v2[i2][:, :], pkv[:, 128:384], [pkv.res], [b_v2[i2].res])
                        cpy("act", b_kf[:, :], pkv[:, 0:128], [pkv.res], [b_kf.res])
                        pgk = PS()
                        mm(pgk[:, 0:128], lrs[0:16, tsl], w2s[0:16, :], True, False, [lrs.res, w2s.res], [pgk.res])
                        mm(pgk[:, 0:128], onesf[0:1, 0:128], bcs[0:1, 0:128], False, True, [onesf.res, bcs.res], [pgk.res])
                        act(b_e1[:, :], pgk[:, 0:128], AF.Exp, [pgk.res], [b_e1.res], scale=-1.0)
                        act(b_sp[:, :], b_e1[:, :], AF.Ln, [b_e1.res], [b_sp.res], bias=1.0)
                        yield
                        bE = b_E2[i2]
                        pB = PS()
                        mm(pB[:, 0:128], b_sp[:, :], triT, True, True, [b_sp.res, cf.res], [pB.res])
                        mm(pB[:, 128:256], triS, b_sp[:, :], True, True, [b_sp.res, cf.res], [pB.res])
                        act(bE[:, 0:128], pB[:, 0:128], AF.Exp, [pB.res], [bE.res], scale=-1.0)
                        act(bE[:, 128:256], pB[:, 0:128], AF.Exp, [pB.res], [bE.res], scale=1.0)
                        act(bE[:, 256:384], pB[:, 128:256], AF.Exp, [pB.res], [bE.res], scale=-1.0)
                        yield
                        stt(b_qt2[i2][:, :], qbs[:, tsl], 32.0 ** -0.5, bE[:, 0:128], ALU.mult, ALU.mult, [qbs.res, bE.res], [b_qt2[i2].res])
                        tt("dve", b_kt[:, :], kbs[:, tsl], bE[:, 128:256], ALU.mult, [kbs.res, bE.res], [b_kt.res])
                        for h in range(4):
                            ts("pool", b_k42[i2][:, h * 128:(h + 1) * 128], b_kt[:, :], headmask[:, h:h + 1], 1.0, ALU.mult, ALU.mult,
                               [b_kt.res, cf.res], [b_k42[i2].res])
                        tt("dve", b_ke2[i2][:, :], b_kf[:, :], bE[:, 256:384], ALU.mult, [b_kf.res, bE.res], [b_ke2[i2].res])
                        bfront_done[(tb, t4)] = True
                        yield

                def genBb(tb, tc):
                    hT = hTs[tb % 2]
                    for t4 in range(4):
                        t0 = t4 * 128
                        tsl = slice(t0, t0 + 128)
                        i2 = t4 % 2
                        while (tb, t4) not in bfront_done:
                            yield
                        bE = b_E2[i2]
                        b_qt = b_qt2[i2]
                        b_k4 = b_k42[i2]
                        b_ke = b_ke2[i2]
                        b_v = b_v2[i2]
                        patt = PS()
                        for h in range(4):
                            mm(patt[:, h * 128:(h + 1) * 128], b_k4[:, h * 128:(h + 1) * 128], b_qt[:, :], True, True,
                               [b_k4.res, b_qt.res], [patt.res])
                        tt("dve", b_at[:, :], patt[:, :], maskLE4[:, :], ALU.mult, [patt.res, maskLE4.res], [b_at.res])
                        yield
                        smp = Sm[schunk[0] % 2]
                        smn = Sm[(schunk[0] + 1) % 2]
                        schunk[0] += 1
                        po = PS()
                        mm(po[:, 0:256], b_qt[:, :], smp[:, :], True, False, [b_qt.res, smp.res], [po.res])
                        for h in range(4):
                            mm(po[:, h * 64:(h + 1) * 64], b_at[:, h * 128:(h + 1) * 128], b_v[:, h * 64:(h + 1) * 64], False, h == 3,
                               [b_at.res, b_v.res], [po.res])
                        pdS = PS()
                        mm(pdS[:, 0:256], b_ke[:, :], b_v[:, :], True, True, [b_ke.res, b_v.res], [pdS.res])
                        stt(Sfull[:, :], Sfull[:, :], bE[:, 127:128], pdS[:, 0:256], ALU.mult, ALU.add, [Sfull.res, bE.res, pdS.res], [Sfull.res])
                        tt("pool", smn[:, :], Sfull[:, :], blockmask, ALU.mult, [Sfull.res, cf.res], [smn.res])
                        pgt = tm(hT, t4, 1024, 256)
                        act(b_osq[:, :], po[:, 0:256], AF.Square, [po.res], [b_osq.res])
                        P.op("dve", lambda e: e.reduce_sum(out=b_s4[:, 0:4], in_=b_osq[:, :].rearrange("p (h e) -> p h e", h=4), axis=AX.X),
                             reads=[b_osq.res], writes=[b_s4.res])
                        act(b_s4[:, 4:8], b_s4[:, 0:4], AF.Ln, [b_s4.res], [b_s4.res], scale=1.0 / 64, bias=EPS)
                        act(b_s4[:, 8:12], b_s4[:, 4:8], AF.Exp, [b_s4.res], [b_s4.res], scale=-0.5)
                        sigm(b_eg, pgt[:, 0:256], [pgt.res])
                        tt("dve", b_gt[:, :], pgt[:, 0:256], b_eg[:, :], ALU.mult, [pgt.res, b_eg.res], [b_gt.res])
                        tt("pool", b_gt[:, :], b_gt[:, :], bcs[:, 128:384], ALU.mult, [b_gt.res, bcs.res], [b_gt.res])
                        for h in range(4):
                            stt(b_ob[:, h * 64:(h + 1) * 64], po[:, h * 64:(h + 1) * 64], b_s4[:, 8 + h:9 + h], b_gt[:, h * 64:(h + 1) * 64],
                                ALU.mult, ALU.mult, [po.res, b_s4.res, b_gt.res], [b_ob.res])
                        yield
                        yield
                        yield
                        yield
                        ptr = PSB()
                        for cc in range(2):
                            P.op("pe", lambda e, cc=cc, ptr=ptr: e.transpose(out=ptr[:, cc * 128:(cc + 1) * 128], in_=b_ob[:, cc * 128:(cc + 1) * 128], identity=identb[:, :]),
                                 reads=[b_ob.res, identb.res], writes=[ptr.res])
                        cpy("act", oBT[:, :, tsl], ptr[:, 0:256].rearrange("p (c t) -> p c t", c=2), [ptr.res], [oBT.res])
                        bback_done[tb * 4 + t4] = True
                        yield
                    dma("sp", cp(oT_d[256:512, :])[:, :, tc], oBT[:, :, :], reads=[oBT.res], writes=[R(ores, (2, tb)), R(ores, (3, tb))])

                def genD(tb, tc):
                    hT = hTs[tb % 2]
                    for t4 in range(4):
                        t0 = t4 * 128
                        tsl = slice(t0, t0 + 128)
                        puv = tm(hT, t4, 2064, 512)
                        cpy("act", d_xs[:, :], puv[:, :], [puv.res], [d_xs.res])
                        yield
                        tt("pool", d_a[:, :], d_xs[:, :], d_xs[:, :], ALU.mult, [d_xs.res], [d_a.res])
                        ts("pool", d_a[:, :], d_a[:, :], 0.044715, 1.0, ALU.mult, ALU.add, [d_a.res], [d_a.res])
                        tt("dve", d_a[:, :], d_a[:, :], d_xs[:, :], ALU.mult, [d_a.res, d_xs.res], [d_a.res])
                        act(d_b[:, :], d_a[:, :], AF.Exp, [d_a.res], [d_b.res], scale=-GELU_K)
                        act(d_b[:, :], d_b[:, :], AF.Ln, [d_b.res], [d_b.res], bias=1.0)
                        act(d_b[:, :], d_b[:, :], AF.Exp, [d_b.res], [d_b.res], scale=-1.0)
                        tt("dve", d_ge[:, :], d_xs[:, :], d_b[:, :], ALU.mult, [d_xs.res, d_b.res], [d_ge.res])
                        yield
                        P.op("dve", lambda e: e.bn_stats(out=d_st[:, 0:6], in_=d_ge[:, 256:512]), reads=[d_ge.res], writes=[d_st.res])
                        P.op("dve", lambda e: e.bn_aggr(out=d_st[:, 8:10], in_=d_st[:, 0:6]), reads=[d_st.res], writes=[d_st.res])
                        act(d_st[:, 10:11], d_st[:, 9:10], AF.Ln, [d_st.res], [d_st.res], bias=EPS)
                        act(d_st[:, 11:12], d_st[:, 10:11], AF.Exp, [d_st.res], [d_st.res], scale=-0.5)
                        ts("dve", d_vn[:, :], d_ge[:, 256:512], d_st[:, 8:9], d_st[:, 11:12], ALU.subtract, ALU.mult, [d_ge.res, d_st.res], [d_vn.res])
                        tt("dve", d_vn[:, :], d_vn[:, :], bcs[:, 384:640], ALU.mult, [d_vn.res, bcs.res], [d_vn.res])
                        tt("pool", d_vl[:, :], d_vn[:, :], bcs[:, 640:896], ALU.add, [d_vn.res, bcs.res], [d_vl.res])
                        yield
                        yield
                        yield
                        psv = PS()
                        for g in range(4):
                            mm(psv[:, g * 64:(g + 1) * 64], sgWT[:, g, :], d_vl[:, g * 64:(g + 1) * 64], True, True, [sgWT.res, d_vl.res], [psv.res])
                        for g in range(4):
                            stt(d_od[:, g * 64:(g + 1) * 64], psv[:, g * 64:(g + 1) * 64], pp[:, o + 132 + g:o + 133 + g], d_ge[:, g * 64:(g + 1) * 64],
                                ALU.add, ALU.mult, [psv.res, pp.res, d_ge.res], [d_od.res])
                        yield
                        yield
                        yield
                        ptr = PSB()
                        for cc in range(2):
                            P.op("pe", lambda e, cc=cc, ptr=ptr: e.transpose(out=ptr[:, cc * 128:(cc + 1) * 128], in_=d_od[:, cc * 128:(cc + 1) * 128], identity=identb[:, :]),
                                 reads=[d_od.res, identb.res], writes=[ptr.res])
                        cpy("act", oDT[:, :, tsl], ptr[:, 0:256].rearrange("p (c t) -> p c t", c=2), [ptr.res], [oDT.res])
                        yield
                    dma("sp", cp(oT_d[768:1024, :])[:, :, tc], oDT[:, :, :], reads=[oDT.res], writes=[R(ores, (6, tb)), R(ores, (7, tb))])

                def genC(tb, tc):
                    hT = hTs[tb % 2]
                    dma("sp", csc[:, :], cs_in[0][:, tc], writes=[csc.res])
                    dma("sp", css[:, :], cs_in[1][:, tc], writes=[css.res])
                    ci = 0
                    for qi, c0 in enumerate((1296, 1552)):
                        r0 = NIN + qi * 256
                        for c in range(2):
                            p1 = fm(hT, c0 + c * 128)
                            tt("dve", c_t1[:, :], p1[:, :], csc[:, :], ALU.mult, [p1.res, csc.res], [c_t1.res])
                            p2 = fm(hT, r0 + c * 128)
                            tt("dve", c_t2[:, :], p2[:, :], css[:, :], ALU.mult, [p2.res, css.res], [c_t2.res])
                            co = c_o[ci % 2]
                            ci += 1
                            tt("pool", co[:, :], c_t1[:, :], c_t2[:, :], ALU.add, [c_t1.res, c_t2.res], [co.res])
                            dma("sp", qkv_d[qi][c * 128:(c + 1) * 128, tc], co[:, :], reads=[co.res], writes=[R(qkvres, (qi, c))])
                            yield
                    for c in range(2):
                        p1 = fm(hT, 1808 + c * 128)
                        co = c_o[ci % 2]
                        ci += 1
                        cpy("act", co[:, :], p1[:, :], [p1.res], [co.res])
                        dma("sp", qkv_d[2][c * 128:(c + 1) * 128, tc], co[:, :], reads=[co.res], writes=[R(qkvres, (2, c))])
                        yield

                def genLN(tb, tc):
                    hT = hTs[tb % 2]
                    dma("sp", xt[:, :, :], cp(xsrc)[:, :, tc], reads=[xres[tb]], writes=[xt.res])
                    yield
                    pssum = PS()
                    for c in range(8):
                        act(sq[:, :], xt[:, c, :], AF.Square, [xt.res], [sq.res])
                        mm(pssum[:, :], onesb[:, :], sq[:, :], c == 0, c == 7, [onesb.res, sq.res], [pssum.res])
                    act(lf[0][:, :], pssum[:, :], AF.Ln, [pssum.res], [lf[0].res], scale=1.0 / D, bias=EPS)
                    act(lf[1][:, :], lf[0][:, :], AF.Exp, [lf[0].res], [lf[1].res], scale=-0.5)
                    yield
                    for c in range(8):
                        f3 = lf[0] if c % 2 == 0 else lf[2]
                        stt(f3[:, :], xt[:, c, :], gm[:, l, c:c + 1], lf[1][:, :], ALU.mult, ALU.mult, [xt.res, lf[1].res, gm.res], [f3.res])
                        ts("pool", hT[:, c, :], f3[:, :], sh1[:, c:c + 1], 1.0, ALU.add, ALU.mult, [f3.res, modt.res], [hT.res])
                        if c % 2 == 1:
                            yield

                makers = {"A": genA, "B": genB, "Bb": genBb, "D": genD, "C": genC}
                order = ("B", "Bb", "D", "A", "C", "LN")
                nxt_tb = {m: 0 for m in makers}
                active = {m: None for m in makers}
                ln_next = 0
                ln_done = -1
                ln_gen = None
                while True:
                    progressed = False
                    for m in makers:
                        if active[m] is None and nxt_tb[m] < NTB and nxt_tb[m] <= ln_done:
                            active[m] = makers[m](nxt_tb[m], tcols(nxt_tb[m]))
                    if ln_gen is None and ln_next < NTB and min(nxt_tb.values()) >= ln_next - 1:
                        ln_gen = genLN(ln_next, tcols(ln_next))
                    for m in order:
                        if m == "LN":
                            if ln_gen is not None:
                                progressed = True
                                try:
                                    next(ln_gen)
                                except StopIteration:
                                    ln_gen = None
                                    ln_done = ln_next
                                    ln_next += 1
                        elif active[m] is not None:
                            progressed = True
                            try:
                                next(active[m])
                            except StopIteration:
                                active[m] = None
                                nxt_tb[m] += 1
                    if not progressed and all(v >= NTB for v in nxt_tb.values()):
                        break
                    assert progressed or ln_gen is not None or any(nxt_tb[m] <= ln_done for m in makers if nxt_tb[m] < NTB), "scheduler stuck"
                P.end_phase()
            if stop_after == (l, 1):
                break

            with ExitStack() as ph:
                qT = sbt(ph, "qT", [128, S], BF16)
                kT = sbt(ph, "kT", [128, S], BF16)
                vT = sbt(ph, "vT", [128, S], BF16)
                acc = [sbt(ph, "acc%d" % i, [128, S], F32) for i in range(2)]
                vz = [sbt(ph, "vz%d" % i, [128, 256], BF16) for i in range(5)]
                Pm = [sbt(ph, "Pm%d" % i, [128, 256], BF16) for i in range(6)]
                rden = sbt(ph, "rden", [128, TB], F32)
                oC = [sbt(ph, "oC%d" % i, [128, TB], BF16) for i in range(2)]
                for z in vz:
                    memset("pool", z[:, :], 1.0, [z.res])
                pmi = 0
                for hp in (p3test["hps"] if p3test else range(2)):
                    dma("sp", qT[:, :], qkv_d[0][hp * 128:(hp + 1) * 128, :], reads=[R(qkvres, (0, hp))], writes=[qT.res])
                    dma("sp", kT[:, :], qkv_d[1][hp * 128:(hp + 1) * 128, :], reads=[R(qkvres, (1, hp))], writes=[kT.res])
                    dma("sp", vT[:, :], qkv_d[2][hp * 128:(hp + 1) * 128, :], reads=[R(qkvres, (2, hp))], writes=[vT.res])
                    vzi = 0
                    pend = []
                    for d in (p3test["branches"] if p3test else (1, 4, 16)):
                        nb = 32 // d
                        for r in range(d):
                            zprev = None
                            for n in range(nb):
                                cur = slice(r + d * 128 * n, r + d * 128 * n + d * 127 + 1, d)
                                prv = slice(r + d * 128 * (n - 1), r + d * 128 * (n - 1) + d * 127 + 1, d) if n > 0 else None
                                zc = vz[vzi % 5]
                                vzi += 1
                                ptr = PSB()
                                P.op("pe", lambda e, ptr=ptr, cur=cur: e.transpose(out=ptr[:, 0:128], in_=vT[:, cur], identity=identb[:, :]),
                                     reads=[vT.res, identb.res], writes=[ptr.res])
                                cpy("act", zc[:, 0:64], ptr[:, 0:64], [ptr.res], [zc.res])
                                cpy("act", zc[:, 192:256], ptr[:, 64:128], [ptr.res], [zc.res])
                                for hh in range(2):
                                    p0 = 64 * hh
                                    pS = PS()
                                    if n > 0:
                                        mm(pS[:, 0:128], kT[p0:p0 + 64, prv], qT[p0:p0 + 64, cur], True, True, [kT.res, qT.res], [pS.res])
                                    mm(pS[:, 128:256], kT[p0:p0 + 64, cur], qT[p0:p0 + 64, cur], True, True, [kT.res, qT.res], [pS.res])
                                    pm_ = Pm[pmi % len(Pm)]
                                    eng = "dve" if pmi % 2 == 0 else "pool"
                                    pmi += 1
                                    lo = 0 if n > 0 else 128
                                    act(pm_[:, lo:256], pS[:, lo:256], AF.Exp, [pS.res], [pm_.res], scale=0.125)
                                    tt(eng, pm_[:, lo:256], pm_[:, lo:256], maskCb[:, lo:256], ALU.mult, [pm_.res, maskCb.res], [pm_.res])

                                    def stage2(n=n, hh=hh, zprev=zprev, zc=zc, pm_=pm_, cur=cur, d=d):
                                        pO = PS()
                                        if n > 0:
                                            mm(pO[:, 0:128], zprev[:, hh * 128:(hh + 1) * 128], pm_[:, 0:128], True, False, [zprev.res, pm_.res], [pO.res])
                                        mm(pO[:, 0:128], zc[:, hh * 128:(hh + 1) * 128], pm_[:, 128:256], n == 0, True, [zc.res, pm_.res], [pO.res])
                                        if d == (p3test["branches"][0] if p3test else 1):
                                            cpy("dve", acc[hh][:, cur], pO[:, 0:128], [pO.res], [acc[hh].res])
                                        else:
                                            tt("dve", acc[hh][:, cur], acc[hh][:, cur], pO[:, 0:128], ALU.add, [acc[hh].res, pO.res], [acc[hh].res])
                                    pend.append(stage2)
                                    while len(pend) > 3:
                                        pend.pop(0)()
                                zprev = zc
                    while pend:
                        pend.pop(0)()
                    for tb in range(NTB):
                        tc = tcols(tb)
                        pden = PS()
                        mm(pden[:, :], swapLo, acc[0][:, tc], True, False, [cf.res, acc[0].res], [pden.res])
                        mm(pden[:, :], swapHi, acc[1][:, tc], False, True, [cf.res, acc[1].res], [pden.res])
                        recip(rden[:, :], pden[:, :], [pden.res], [rden.res])
                        oc = oC[tb % 2]
                        tt("dve", oc[0:64, :], acc[0][0:64, tc], rden[0:64, :], ALU.mult, [acc[0].res, rden.res], [oc.res])
                        tt("dve", oc[64:128, :], acc[1][64:128, tc], rden[64:128, :], ALU.mult, [acc[1].res, rden.res], [oc.res])
                        dma("sp", oT_d[512 + hp * 128:512 + (hp + 1) * 128, tc], oc[:, :], reads=[oc.res], writes=[R(ores, (4 + hp, tb))])
                P.end_phase()
            if stop_after == (l, 3):
                break

            for f in range(2):
                with ExitStack() as ph:
                    xts = [sbt(ph, "xt%d" % i, [128, 8, TB], F32) for i in range(2)]
                    h2Ts = [sbt(ph, "h2T%d" % i, [128, 8, TB], BF16) for i in range(2)]
                    gTs = [sbt(ph, "gT%d" % i, [128, 11, TB], BF16) for i in range(2)]
                    yb = [[sbt(ph, "yb%d_%d" % (h_, i), [128, TB + 2], F32) for i in range(2)] for h_ in range(2)]
                    halo = sbt(ph, "halo", [128, 22, 2], F32)
                    cc_ = [[sbt(ph, "cc%d_%d" % (h_, i), [128, TB], F32) for i in range(2)] for h_ in range(2)]
                    f_es = [sbt(ph, "f_e%d" % i, [128, TB], F32) for i in range(2)]
                    f_ss = [sbt(ph, "f_s%d" % i, [128, TB], F32) for i in range(2)]
                    if f == 0:
                        wo = sbt(ph, "wo", [128, 8, D], BF16)
                        oTt = sbt(ph, "oTt", [128, 8, TB], BF16)
                        sq = sbt(ph, "sq", [128, TB], BF16)
                        lf = [sbt(ph, "lf%d" % i, [128, TB], F32) for i in range(3)]
                        wor = [Res() for _ in range(8)]
                        for k in range(8):
                            dma("pool", wo[:, k, :], w_out[l][k * 128:(k + 1) * 128, :], writes=[wor[k]])
                    for k in range(8):
                        dma("pool", ws_up[:, k, 0:1408], ffn_up[l][k * 128:(k + 1) * 128, f * 1408:(f + 1) * 1408], writes=[wsr[2 * k]])
                        dma("pool", ws_up[:, k, 1408:2816], ffn_up[l][k * 128:(k + 1) * 128, DFF + f * 1408:DFF + (f + 1) * 1408], writes=[wsr[2 * k + 1]])
                    for jj in range(11):
                        dma("pool", ws_dn[:, jj, :], ffn_down[l][(f * 11 + jj) * 128:(f * 11 + jj + 1) * 128, :], writes=[wsr[16 + jj]])
                    memset("pool", halo[:, :, :], 0.0, [halo.res])

                    def stage_in(tb):
                        tc = tcols(tb)
                        xt = xts[tb % 2]
                        h2T = h2Ts[tb % 2]
                        if f == 0:
                            dma("sp", xt[:, :, :], cp(xsrc)[:, :, tc], reads=[xres[tb]], writes=[xt.res])
                            dma("sp", oTt[:, :, :], cp(oT_d)[:, :, tc], reads=[R(ores, (c, tb)) for c in range(8)], writes=[oTt.res])
                            for m in range(8):
                                pm = PS()
                                for kc in range(8):
                                    mm(pm[:, :], wo[:, kc, m * 128:(m + 1) * 128], oTt[:, kc, :], kc == 0, kc == 7, wor + [oTt.res], [pm.res])
                                stt(xt[:, m, :], pm[:, :], g1[:, m:m + 1], xt[:, m, :], ALU.mult, ALU.add, [pm.res, modt.res, xt.res], [xt.res])
                            rmsnorm_fm(xt, gm[:, l, 8:16], sh2, h2T, False, sq, lf[0], lf[1], lf[2])
                            dma("sp", cp(h2T_d)[:, :, tc], h2T[:, :, :], reads=[h2T.res], writes=[h2res[tb]])
                        else:
                            dma("sp", h2T[:, :, :], cp(h2T_d)[:, :, tc], reads=[h2res[tb]], writes=[h2T.res])
                            dma("sp", xt[:, :, :], cp(xT_d)[:, :, tc], reads=[xres[tb]], writes=[xt.res])

                    pend = []
                    pi = 0
                    stage_in(0)
                    for tb in range(NTB):
                        tc = tcols(tb)
                        xt = xts[tb % 2]
                        h2T = h2Ts[tb % 2]
                        gT = gTs[tb % 2]
                        for jj in range(11):
                            cvs = []
                            for half in range(2):
                                ch = half * 22 + f * 11 + jj
                                hid = half * 11 + jj
                                pz = PS()
                                for k in range(8):
                                    mm(pz[:, :], ws_up[:, k, half * 1408 + jj * 128:half * 1408 + (jj + 1) * 128], h2T[:, k, :], k == 0, k == 7,
                                       wsall + [h2T.res], [pz.res])
                                y = yb[half][pi % 2]
                                cv = cc_[half][pi % 2]
                                cvs.append(cv)
                                cw = o + 136 + ch * 3
                                cpy("pool", y[:, 0:2], halo[:, hid, :], [halo.res], [y.res])
                                cpy("act", y[:, 2:TB + 2], pz[:, :], [pz.res], [y.res])
                                act(cv[:, :], pz[:, :], AF.Identity, [pz.res, pp.res], [cv.res],
                                    scale=pp[:, cw + 2:cw + 3], bias=pp[:, o + 268 + ch:o + 269 + ch])
                                cpy("pool", halo[:, hid, :], y[:, TB:TB + 2], [y.res], [halo.res])
                                stt(cv[:, :], y[:, 1:TB + 1], pp[:, cw + 1:cw + 2], cv[:, :], ALU.mult, ALU.add, [y.res, pp.res, cv.res], [cv.res])
                                stt(cv[:, :], y[:, 0:TB], pp[:, cw:cw + 1], cv[:, :], ALU.mult, ALU.add, [y.res, pp.res, cv.res], [cv.res])
                            f_e = f_es[pi % 2]
                            f_s = f_ss[pi % 2]
                            pi += 1
                            act(f_e[:, :], cvs[0][:, :], AF.Exp, [cvs[0].res], [f_e.res], scale=-1.0)
                            act(f_e[:, :], f_e[:, :], AF.Ln, [f_e.res], [f_e.res], bias=1.0)
                            act(f_e[:, :], f_e[:, :], AF.Exp, [f_e.res], [f_e.res], scale=-1.0)
                            tt("dve", f_s[:, :], cvs[0][:, :], f_e[:, :], ALU.mult, [cvs[0].res, f_e.res], [f_s.res])
                            tt("dve", gT[:, jj, :], f_s[:, :], cvs[1][:, :], ALU.mult, [f_s.res, cvs[1].res], [gT.res])
                            if jj == 2:
                                while pend:
                                    pend.pop(0)()
                            if jj == 5 and tb + 1 < NTB:
                                stage_in(tb + 1)

                        def down(tb=tb, tc=tc, xt=xt, gT=gT):
                            for m in range(8):
                                pd = PS()
                                for jj in range(11):
                                    mm(pd[:, :], ws_dn[:, jj, m * 128:(m + 1) * 128], gT[:, jj, :], jj == 0, jj == 10, wsall + [gT.res], [pd.res])
                                stt(xt[:, m, :], pd[:, :], g2[:, m:m + 1], xt[:, m, :], ALU.mult, ALU.add, [pd.res, modt.res, xt.res], [xt.res])
                            dma("sp", cp(xT_d)[:, :, tc], xt[:, :, :], reads=[xt.res], writes=[xres[tb]])
                        pend.append(down)
                    while pend:
                        pend.pop(0)()
                    P.end_phase()

        if stop_after is None and final:
            with ExitStack() as ph:
                xt = sbt(ph, "xt", [128, 8, TB], F32)
                ot = sbt(ph, "ot", [128, 8, TB], F32)
                sq = sbt(ph, "sq", [128, TB], BF16)
                lf = [sbt(ph, "lf%d" % i, [128, TB], F32) for i in range(3)]
                xfin = xT_in if nlayers == 0 else xT_d
                for tb in range(NTB):
                    tc = tcols(tb)
                    dma("sp", xt[:, :, :], cp(xfin)[:, :, tc], reads=[xres[tb]], writes=[xt.res])
                    rmsnorm_fm(xt, pp[:, L * PPL:L * PPL + 8], None, ot, True, sq, lf[0], lf[1], lf[2])
                    dma("sp", cp(outT)[:, :, tc], ot[:, :, :], reads=[ot.res], writes=[Res()])
                P.end_phase()
        print("total ops", P.total_ops, "max sem count/phase", P.maxcnt, P.dcnt)
    return nc


_CACHE = {}
NSPLIT = 1


def kernel(**inputs):
    inp = {k: np.ascontiguousarray(np.asarray(v, dtype=np.float32)) for k, v in inputs.items()}
    pp = _pack_pp(inp)
    bc = _pack_bc(inp)
    cf, cs = _consts()
    B = inp["x"].shape[0]
    per = L // NSPLIT
    xTs = [np.ascontiguousarray(inp["x"][b].T) for b in range(B)]
    for s in range(NSPLIT):
        last = (s == NSPLIT - 1)
        key = ("prog", s, NSPLIT)
        if key not in _CACHE:
            _CACHE[key] = build_program(nlayers=per, l0=s * per, final=last)
        nc = _CACHE[key]
        in_maps = []
        for b in range(B):
            in_maps.append({
                "xT": xTs[b],
                "cT": np.ascontiguousarray(inp["c"][b].reshape(8, 128).T),
                "ada_w": inp["ada_w"], "w_in": inp["w_in"], "w_out": inp["w_out"],
                "ffn_up": inp["ffn_up"], "ffn_down": inp["ffn_down"],
                "gla_w2": inp["gla_w2"], "sgu_w": inp["sgu_w"],
                "pp": pp, "bc": bc, "cf": cf, "cs": cs,
            })
        res = run_bass_kernel_spmd(nc, in_maps, core_ids=list(range(B)))
        if last:
            out = np.stack([np.ascontiguousarray(r["outT"].T) for r in res.results], axis=0)
        else:
            xTs = [np.ascontiguousarray(np.asarray(r["xT_d"], dtype=np.float32)) for r in res.results]
    return out.astype(np.float32)
```
